# Optimizing a Trainium2 kernel written in Bass

```python
import math, functools
import jax, jax.numpy as jnp
from jax import lax
import numpy as np

D_MODEL = 1024
BATCH = 2
SEQ = 8192
DEPTH = 1

GRID_W = 64
D_MIX = D_MODEL
HEAD_DIM = 64
N_Q_HEADS = 8
N_KV_HEADS = 2
GQA_GROUP = N_Q_HEADS // N_KV_HEADS
ATTN_W = N_Q_HEADS * HEAD_DIM
KV_W = N_KV_HEADS * HEAD_DIM
LRU_W = D_MIX - ATTN_W
LRU_BLOCKS = 8
LRU_BW = LRU_W // LRU_BLOCKS
LRU_C = 8.0
CONV_W = 4
CONV_PAD_L = 2
ROPE_THETA = 10000.0
ROPE_HALF = HEAD_DIM // 2
Q_BLOCK = 128
N_EXPERTS = 32
TOP_K = 4
D_FF = D_MODEL
SWIGLU_ALPHA = 1.702
SWIGLU_LIMIT = 7.0
MOE_BLOCK = 256
NORM_EPS = 1e-5
IN_W = ATTN_W + 2 * KV_W + 2 * LRU_W

kernel_name = 'hybrid_attn_rglru_moe_encoder'


def rms_norm(x, g, eps=NORM_EPS):
    xf = x.astype(jnp.float32)
    y = xf * lax.rsqrt(jnp.mean(xf * xf, axis=-1, keepdims=True) + eps)
    return (y * g.astype(jnp.float32)).astype(x.dtype)


def rope_1d(x, pos):
    m = x.shape[-1] // 2
    inv_freq = ROPE_THETA ** (-jnp.arange(m, dtype=jnp.float32) / m)
    ang = pos.astype(jnp.float32)[:, None] * inv_freq[None, :]
    cos = jnp.cos(ang)[None, :, None, :]
    sin = jnp.sin(ang)[None, :, None, :]
    xf = x.astype(jnp.float32)
    x1, x2 = xf[..., :m], xf[..., m:]
    out = jnp.concatenate([x1 * cos - x2 * sin, x2 * cos + x1 * sin], axis=-1)
    return out.astype(x.dtype)


def rope_axial_2d(x, rows, cols):
    return jnp.concatenate([rope_1d(x[..., :ROPE_HALF], rows),
                            rope_1d(x[..., ROPE_HALF:], cols)], axis=-1)


def block_attention(q, k, v):
    B, S = q.shape[0], q.shape[1]
    nblk = S // Q_BLOCK
    qb = q.reshape(B, nblk, Q_BLOCK, N_KV_HEADS, GQA_GROUP, HEAD_DIM)
    qb = jnp.moveaxis(qb, 1, 0)
    scale = HEAD_DIM ** -0.5

    def one_block(qi):
        s = jnp.einsum('bqkgd,bskd->bkgqs', qi, k).astype(jnp.float32) * scale
        p = jax.nn.softmax(s, axis=-1).astype(v.dtype)
        return jnp.einsum('bkgqs,bskd->bqkgd', p, v)

    o = lax.map(one_block, qb)
    return jnp.moveaxis(o, 0, 1).reshape(B, S, ATTN_W)


def centred_depthwise_conv(u, w, b):
    S = u.shape[1]
    up = jnp.pad(u, ((0, 0), (CONV_PAD_L, CONV_W - 1 - CONV_PAD_L), (0, 0)))
    acc = b
    for j in range(CONV_W):
        acc = acc + up[:, j:j + S, :] * w[j]
    return acc


def _lin_combine(c1, c2):
    a1, b1 = c1
    a2, b2 = c2
    return a1 * a2, a2 * b1 + b2


def rglru_direction(xc, wa, ba, wi, bi, lam, reverse):
    B, S, _ = xc.shape
    xb = xc.reshape(B, S, LRU_BLOCKS, LRU_BW)
    r = jax.nn.sigmoid((jnp.einsum('bsni,nij->bsnj', xb, wa).reshape(B, S, LRU_W) + ba).astype(jnp.float32))
    i = jax.nn.sigmoid((jnp.einsum('bsni,nij->bsnj', xb, wi).reshape(B, S, LRU_W) + bi).astype(jnp.float32))
    log_a = -LRU_C * r * jax.nn.softplus(-lam.astype(jnp.float32))
    a = jnp.exp(log_a)
    mult = jnp.sqrt(-jnp.expm1(2.0 * log_a))
    bterm = mult * i * xc.astype(jnp.float32)
    _, h = lax.associative_scan(_lin_combine, (a, bterm), reverse=reverse, axis=1)
    return h


def moe_ffn(xn, w_router, b_router, w_gate, b_gate, w_up, b_up, w_down, b_down):
    B, S, D = xn.shape
    T = B * S
    xf = xn.reshape(T, D)
    logits = (xf @ w_router + b_router).astype(jnp.float32)
    top_v, top_i = lax.top_k(logits, TOP_K)
    gates = jax.nn.softmax(top_v, axis=-1).astype(xn.dtype)

    TK = T * TOP_K
    flat_e = top_i.reshape(TK).astype(jnp.int32)
    flat_tok = jnp.arange(TK, dtype=jnp.int32) // TOP_K
    flat_g = gates.reshape(TK)
    order = jnp.argsort(flat_e, stable=True)
    sorted_e = flat_e[order]
    counts = jnp.bincount(flat_e, length=N_EXPERTS)
    padded = ((counts + MOE_BLOCK - 1) // MOE_BLOCK) * MOE_BLOCK
    start = jnp.cumsum(counts) - counts
    pend = jnp.cumsum(padded)
    pstart = pend - padded
    dest = pstart[sorted_e] + (jnp.arange(TK, dtype=jnp.int32) - start[sorted_e])

    n_rows = TK + N_EXPERTS * MOE_BLOCK
    n_blocks = n_rows // MOE_BLOCK
    row_tok = jnp.full((n_rows,), T, dtype=jnp.int32).at[dest].set(flat_tok[order])
    row_gate = jnp.zeros((n_rows,), dtype=xn.dtype).at[dest].set(flat_g[order])
    block_e = jnp.searchsorted(pend, jnp.arange(n_blocks) * MOE_BLOCK, side='right')
    block_e = jnp.minimum(block_e, N_EXPERTS - 1).astype(jnp.int32)

    xpad = jnp.concatenate([xf, jnp.zeros((1, D), xf.dtype)], axis=0)
    x_rows = xpad[row_tok].reshape(n_blocks, MOE_BLOCK, D)

    def expert_block(args):
        xb, e = args
        g = xb @ w_gate[e] + b_gate[e]
        u = xb @ w_up[e] + b_up[e]
        g = jnp.minimum(g, SWIGLU_LIMIT)
        u = jnp.clip(u, -SWIGLU_LIMIT, SWIGLU_LIMIT)
        glu = g * jax.nn.sigmoid(SWIGLU_ALPHA * g)
        return ((u + 1.0) * glu) @ w_down[e] + b_down[e]

    y_rows = lax.map(expert_block, (x_rows, block_e)).reshape(n_rows, D)
    y_rows = y_rows * row_gate[:, None]
    out = jax.ops.segment_sum(y_rows, row_tok, num_segments=T + 1)[:T]
    return out.reshape(B, S, D)


def setup_inputs(seed: int = 0) -> dict:
    key = jax.random.key(seed)
    ks = jax.random.split(key, 32)
    f32 = jnp.float32
    L = DEPTH

    def nrm(k, shape, fan_in):
        return jax.random.normal(k, shape, f32) * (fan_in ** -0.5)

    def gain(k, shape):
        return 1.0 + 0.05 * jax.random.normal(k, shape, f32)

    def bias(k, shape, s=0.01):
        return s * jax.random.normal(k, shape, f32)

    u = jax.random.uniform(ks[10], (L, 2, LRU_W), f32, minval=0.9, maxval=0.999)
    sgm = u ** (1.0 / LRU_C)
    lru_lam = jnp.log(sgm) - jnp.log1p(-sgm)

    return {
        'x': jax.random.normal(ks[0], (BATCH, SEQ, D_MODEL), f32),
        'norm1_g': gain(ks[1], (L, D_MODEL)),
        'w_in': nrm(ks[2], (L, D_MODEL, IN_W), D_MODEL),
        'q_norm_g': gain(ks[3], (L, HEAD_DIM)),
        'k_norm_g': gain(ks[4], (L, HEAD_DIM)),
        'conv_w': nrm(ks[5], (L, CONV_W, LRU_W), CONV_W),
        'conv_b': bias(ks[6], (L, LRU_W)),
        'lru_wa': nrm(ks[7], (L, 2, LRU_BLOCKS, LRU_BW, LRU_BW), LRU_BW),
        'lru_ba': bias(ks[8], (L, 2, LRU_W), 0.1),
        'lru_wi': nrm(ks[9], (L, 2, LRU_BLOCKS, LRU_BW, LRU_BW), LRU_BW),
        'lru_bi': bias(ks[11], (L, 2, LRU_W), 0.1),
        'lru_lam': lru_lam,
        'attn_out_g': gain(ks[12], (L, ATTN_W)),
        'lru_out_g': gain(ks[13], (L, LRU_W)),
        'w_out': nrm(ks[14], (L, D_MIX, D_MODEL), D_MIX),
        'norm2_g': gain(ks[15], (L, D_MODEL)),
        'w_router': nrm(ks[16], (L, D_MODEL, N_EXPERTS), D_MODEL),
        'b_router': bias(ks[17], (L, N_EXPERTS)),
        'w_gate': nrm(ks[18], (L, N_EXPERTS, D_MODEL, D_FF), D_MODEL),
        'b_gate': bias(ks[19], (L, N_EXPERTS, D_FF)),
        'w_up': nrm(ks[20], (L, N_EXPERTS, D_MODEL, D_FF), D_MODEL),
        'b_up': bias(ks[21], (L, N_EXPERTS, D_FF)),
        'w_down': nrm(ks[22], (L, N_EXPERTS, D_FF, D_MODEL), D_FF),
        'b_down': bias(ks[23], (L, N_EXPERTS, D_MODEL)),
        'final_g': gain(ks[24], (D_MODEL,)),
    }


def reference(x, norm1_g, w_in, q_norm_g, k_norm_g, conv_w, conv_b, lru_wa, lru_ba,
              lru_wi, lru_bi, lru_lam, attn_out_g, lru_out_g, w_out, norm2_g,
              w_router, b_router, w_gate, b_gate, w_up, b_up, w_down, b_down, final_g):
    B, S, _ = x.shape
    n_rows_grid = S // GRID_W
    rows = jnp.broadcast_to(jnp.arange(n_rows_grid)[:, None], (n_rows_grid, GRID_W)).reshape(S)
    cols = jnp.broadcast_to(jnp.arange(GRID_W)[None, :], (n_rows_grid, GRID_W)).reshape(S)

    for l in range(DEPTH):
        xn = rms_norm(x, norm1_g[l])
        h_in = xn @ w_in[l]
        o0, o1, o2, o3 = ATTN_W, ATTN_W + KV_W, ATTN_W + 2 * KV_W, ATTN_W + 2 * KV_W + LRU_W
        q = h_in[..., :o0].reshape(B, S, N_Q_HEADS, HEAD_DIM)
        k = h_in[..., o0:o1].reshape(B, S, N_KV_HEADS, HEAD_DIM)
        v = h_in[..., o1:o2].reshape(B, S, N_KV_HEADS, HEAD_DIM)
        lru_x = h_in[..., o2:o3]
        lru_gate = h_in[..., o3:]

        q = rope_axial_2d(rms_norm(q, q_norm_g[l], 1e-6), rows, cols)
        k = rope_axial_2d(rms_norm(k, k_norm_g[l], 1e-6), rows, cols)
        attn = block_attention(q, k, v)

        xc = centred_depthwise_conv(lru_x, conv_w[l], conv_b[l])
        h_f = rglru_direction(xc, lru_wa[l, 0], lru_ba[l, 0], lru_wi[l, 0], lru_bi[l, 0],
                              lru_lam[l, 0], False)
        h_b = rglru_direction(xc, lru_wa[l, 1], lru_ba[l, 1], lru_wi[l, 1], lru_bi[l, 1],
                              lru_lam[l, 1], True)
        lru = ((h_f + h_b).astype(x.dtype)) * jax.nn.gelu(lru_gate)

        mixed = jnp.concatenate([rms_norm(attn, attn_out_g[l]),
                                 rms_norm(lru, lru_out_g[l])], axis=-1)
        x = x + mixed @ w_out[l]

        x = x + moe_ffn(rms_norm(x, norm2_g[l]), w_router[l], b_router[l], w_gate[l],
                        b_gate[l], w_up[l], b_up[l], w_down[l], b_down[l])

    return rms_norm(x, final_g)
```

```python
import math
import numpy as np
from contextlib import ExitStack
import concourse.bass as bass
import concourse.mybir as mybir
from concourse.alu_op_type import AluOpType as ALU
from concourse.bass_utils import run_bass_kernel_spmd

F32 = mybir.dt.float32
BF16 = mybir.dt.bfloat16
I32 = mybir.dt.int32
AF = mybir.ActivationFunctionType
AX = mybir.AxisListType

S = 8192
D = 1024
NE = 32
CAP = 1024
SUBC = 512
NSLOT = NE * CAP
SB_BYTES = 180224


class Buf:
    __slots__ = ("w", "r", "x")

    def __init__(self, x=False):
        self.w = None
        self.r = []
        self.x = x


class FW:
    def __init__(self, nc, ctx, n_dma_sems=40):
        self.nc = nc
        self.eng = {"pe": nc.tensor, "dve": nc.vector, "act": nc.scalar, "pool": nc.gpsimd, "sp": nc.sync}
        self.psem, self.cnt, self.seen = {}, {}, {}
        for e in self.eng:
            self.psem[e] = ctx.enter_context(nc.semaphore("ps_" + e))
            self.cnt[e] = 0
            self.seen[e] = {}
        self.dsems = [ctx.enter_context(nc.semaphore("dq%d" % i)) for i in range(n_dma_sems)]
        self.dval = [0] * n_dma_sems
        self.dnext = 0
        self.prog = {e: [] for e in self.eng}
        self.nops = 0
        self.limit = None
        self.log = []

    def _wait(self, e, ev):
        if ev is None:
            return
        sem, val = ev
        k = id(sem)
        if self.seen[e].get(k, 0) >= val:
            return
        self.prog[e].append(("w", sem, val))
        self.seen[e][k] = val

    def _deps(self, e, reads, writes):
        own = id(self.psem[e])
        for b in reads:
            ev = b.w
            if ev is not None and not (e == "pe" and id(ev[0]) == own):
                self._wait(e, ev)
            if b.x:
                for ev in b.r:
                    if id(ev[0]) != own:
                        self._wait(e, ev)
        for b in writes:
            ev = b.w
            if ev is not None and not (e == "pe" and id(ev[0]) == own):
                self._wait(e, ev)
            for ev in b.r:
                if not (e == "pe" and id(ev[0]) == own):
                    self._wait(e, ev)

    def _mark(self, ev, reads, writes):
        for b in reads:
            b.r.append(ev)
            if len(b.r) > 16:
                d = {}
                for s, v in b.r:
                    if id(s) not in d or d[id(s)][1] < v:
                        d[id(s)] = (s, v)
                b.r = list(d.values())
        for b in writes:
            b.w = ev
            b.r = []

    def op(self, e, fn, reads=(), writes=()):
        self.nops += 1
        if self.limit is not None and self.nops > self.limit:
            return None
        self._deps(e, reads, writes)
        self.cnt[e] += 1
        self.prog[e].append(("i", fn, self.psem[e], 1))
        ev = (self.psem[e], self.cnt[e])
        self._mark(ev, reads, writes)
        return ev

    def dma(self, e, fn, reads=(), writes=()):
        self.nops += 1
        if self.limit is not None and self.nops > self.limit:
            return None
        self._deps(e, reads, writes)
        i = self.dnext
        self.dnext = (self.dnext + 1) % len(self.dsems)
        sem = self.dsems[i]
        if self.dval[i] > 0:
            self._wait(e, (sem, self.dval[i]))
        self.dval[i] += 16
        self.prog[e].append(("i", fn, sem, 16))
        ev = (sem, self.dval[i])
        self._mark(ev, reads, writes)
        return ev

    def barrier(self, engines=None):
        for e in (engines or self.eng):
            for f in self.eng:
                if f != e and self.cnt[f] > 0:
                    self._wait(e, (self.psem[f], self.cnt[f]))
            for i, sm in enumerate(self.dsems):
                if self.dval[i] > 0:
                    self._wait(e, (sm, self.dval[i]))

    def finish(self):
        self.barrier(["sp"])
        nc, prog = self.nc, self.prog

        def emit(engine, lst):
            for it in lst:
                if it[0] == "w":
                    engine.wait_ge(it[1], it[2])
                else:
                    it[1](engine).then_inc(it[2], it[3])

        with nc.Block() as block:
            @block.sync
            def _(x):
                emit(x, prog["sp"])

            @block.tensor
            def _(x):
                emit(x, prog["pe"])

            @block.vector
            def _(x):
                emit(x, prog["dve"])

            @block.scalar
            def _(x):
                emit(x, prog["act"])

            @block.gpsimd
            def _(x):
                emit(x, prog["pool"])


PRM = {}
_o = 0
for _n, _w in [("g1c", 8), ("gq_bc", 64), ("gk_bc", 64), ("gq_col", 1), ("gqp_col", 1), ("gk_col", 1), ("gkp_col", 1),
               ("convw", 16), ("convb", 4), ("ba", 8), ("bi", 8), ("lam", 8), ("gao", 4), ("glo", 4), ("brt", 32),
               ("g2", 1024), ("gf", 1024), ("bg", 256), ("bu", 256)]:
    PRM[_n] = (_o, _w)
    _o += _w
NPRM = _o
CST = {}
_o = 0
for _n, _w in [("identf", 128), ("ustrict", 128), ("onesf", 128), ("rtab", 384), ("msk", 8), ("iota1", 32), ("slot1", 32)]:
    CST[_n] = (_o, _w)
    _o += _w
NCST = _o


def _esz(dt):
    return 2 if dt == BF16 else 4


def build_nc(dbg=False, stop=None, limit=None):
    nc = bass.Bass("TRN2", target_bir_lowering=False)

    def din(name, shape, dt=F32):
        return nc.dram_tensor(name, list(shape), dt, kind="ExternalInput").ap()

    xs = din("xs", [S, D])
    prm_d = din("prm", [128, NPRM])
    cst_d = din("cst", [128, NCST])
    cmat_d = din("cmat", [128, 384])
    w_in_d = din("w_in", [D, 1792])
    wabd_d = din("wabd", [128, 1024])
    wibd_d = din("wibd", [128, 1024])
    w_out_d = din("w_out", [D, D])
    w_router_d = din("w_router", [D, NE])
    if stop is None or stop == "E":
        w_gate_d = din("w_gate", [NE, D, D])
        w_up_d = din("w_up", [NE, D, D])
        w_down_d = din("w_down", [NE, D, D])
        b_down_d = din("b_down", [NE, D])
    y_d = nc.dram_tensor("y", [2048, D], F32, kind="ExternalOutput").ap()
    scr_lru = nc.dram_tensor("scr_lru", [4, 128, S + 4], BF16, kind="Internal").ap()
    scr_gg = nc.dram_tensor("scr_gg", [4, 128, 2048], F32, kind="Internal").ap()
    scr_x1 = nc.dram_tensor("scr_x1", [2048, D], F32, kind="Internal").ap()
    scr_xd = nc.dram_tensor("scr_xd", [NSLOT, D], F32, kind="Internal").ap()
    scr_y = nc.dram_tensor("scr_y", [NSLOT, D], F32, kind="Internal").ap()
    dbg_out = {}
    if dbg:
        dbg_out["d_kT"] = nc.dram_tensor("d_kT", [128, S], BF16, kind="ExternalOutput").ap()
        dbg_out["d_qT"] = nc.dram_tensor("d_qT", [128, 4 * 2048], BF16, kind="ExternalOutput").ap()
        dbg_out["d_V"] = nc.dram_tensor("d_V", [128, 64 * 130], BF16, kind="ExternalOutput").ap()
        dbg_out["d_attn"] = nc.dram_tensor("d_attn", [128, 16 * 512], F32, kind="ExternalOutput").ap()
        dbg_out["d_lru"] = nc.dram_tensor("d_lru", [128, 4 * 2048], BF16, kind="ExternalOutput").ap()
        dbg_out["d_x1"] = nc.dram_tensor("d_x1", [2048, D], F32, kind="ExternalOutput").ap()

    with ExitStack() as ctx:
        fw = FW(nc, ctx)
        fw.limit = limit
        nc._fw = fw
        SB = ctx.enter_context(nc.sbuf_tensor("SB", [128, SB_BYTES // 4], F32))
        pb = [ctx.enter_context(nc.psum_tensor("pb%d" % i, [128, 512], F32))[:] for i in range(8)]
        pbb = [Buf(True) for _ in range(8)]

        def V(off, shape, dt=F32):
            n = int(np.prod(shape[1:]))
            nb = n * _esz(dt)
            assert off % 4 == 0 and nb % 4 == 0 and off + nb <= SB_BYTES, (off, shape)
            ap = SB[:, off // 4:(off + nb) // 4]
            if dt != F32:
                ap = ap.bitcast(dt)
            if len(shape) > 2:
                names = "abcd"[:len(shape) - 1]
                ap = ap.rearrange("p (%s) -> p %s" % (" ".join(names), " ".join(names)),
                                  **{k: int(s) for k, s in zip(names, shape[1:])})
            return ap

        def ACT(out, in_, func, reads, writes, scale=None, bias=None, accum=None):
            kw = {}
            if scale is not None:
                kw["scale"] = scale
            if bias is not None:
                kw["bias"] = bias
            if accum is not None:
                kw["accum_out"] = accum
            return fw.op("act", lambda e: e.activation(out=out, in_=in_, func=func, **kw), reads, writes)

        def TS(out, in0, s1, op0, reads, writes, s2=None, op1=None, eng="dve", accum=None):
            kw = {}
            if op1 is not None:
                kw["op1"] = op1
            if accum is not None:
                kw["accum_out"] = accum
            return fw.op(eng, lambda e: e.tensor_scalar(out=out, in0=in0, scalar1=s1, scalar2=s2, op0=op0, **kw), reads, writes)

        def TT(out, in0, in1, op, reads, writes, eng="dve"):
            return fw.op(eng, lambda e: e.tensor_tensor(out=out, in0=in0, in1=in1, op=op), reads, writes)

        def STT(out, in0, scalar, in1, op0, op1, reads, writes, accum=None):
            kw = {}
            if accum is not None:
                kw["accum_out"] = accum
            return fw.op("dve", lambda e: e.scalar_tensor_tensor(out=out, in0=in0, scalar=scalar, in1=in1, op0=op0, op1=op1, **kw),
                         reads, writes)

        def CP(eng, out, in_, reads, writes):
            if eng == "act":
                return fw.op("act", lambda e: e.copy(out=out, in_=in_), reads, writes)
            return fw.op(eng, lambda e: e.tensor_copy(out=out, in_=in_), reads, writes)

        def MM(out, lhsT, rhs, start, stop, reads, writes, skip=False):
            return fw.op("pe", lambda e: e.matmul(out, lhsT=lhsT, rhs=rhs, start=start, stop=stop, skip_group_check=skip), reads, writes)

        def TR(out, in_, ident, reads, writes):
            return fw.op("pe", lambda e: e.transpose(out=out, in_=in_, identity=ident), reads, writes)

        def DMA(eng, out, in_, reads, writes, slow=False):
            if slow:
                return fw.dma(eng, lambda e: e.dma_start(out=out, in_=in_, allow_slow_non_contiguous=True), reads, writes)
            return fw.dma(eng, lambda e: e.dma_start(out=out, in_=in_), reads, writes)

        def RSTD(out, ssq_ap, inv_n, eps, tmp, reads, writes):
            ACT(tmp, ssq_ap, AF.Ln, reads, writes, scale=inv_n, bias=eps)
            ACT(out, tmp, AF.Exp, writes, writes, scale=-0.5)

        o = 0
        prm = V(o, [128, NPRM]); o += NPRM * 4; prm_b = Buf()
        cst = V(o, [128, NCST]); o += NCST * 4; cst_b = Buf()
        cmat = V(o, [128, 384], BF16); o += 768; cmat_b = Buf()
        small = V(o, [128, 256]); o += 1024; small_b = Buf()
        colt = V(o, [128, 4, 64]); o += 1024; colt_b = Buf()
        diagw = V(o, [128, 16, 128], BF16); o += 4096; diagw_b = Buf()
        wabd = V(o, [128, 8, 128], BF16); o += 2048
        wibd = V(o, [128, 8, 128], BF16); o += 2048; wbd_b = Buf()
        wr_f = V(o, [128, 8, NE]); o += 1024; wr_b = Buf()
        carry = V(o, [128, 8]); o += 32; carry_b = [Buf() for _ in range(8)]
        rstd_a = V(o, [128, 16]); o += 64; rstda_b = Buf()
        cm = V(o, [128, NE]); o += 128; cm_b = Buf()
        gate4 = V(o, [128, 16, 4]); o += 256; gate4_b = Buf()
        dest4 = V(o, [128, 16, 4], I32); o += 256; dest4_b = Buf()
        ones_bf = V(o, [128, 2], BF16); o += 4; ones_b = Buf()
        o = (o + 31) // 32 * 32
        junk = V(o, [128, 1024], BF16); o += 2048; junk_b = Buf()
        sc = V(o, [128, 64]); o += 256
        sc_b = [Buf() for _ in range(64)]
        PH0 = (o + 63) // 64 * 64

        def P(name):
            a, w = PRM[name]
            return prm[:, a:a + w]

        def Cc(name):
            a, w = CST[name]
            return cst[:, a:a + w]

        identf = Cc("identf")
        ident_bf = cmat[:, 0:128]
        perm_bf = cmat[:, 128:256]
        bones_bf = cmat[:, 256:384]
        rtab = Cc("rtab")
        msk = Cc("msk")

        DMA("sp", prm, prm_d[:, :], [], [prm_b])
        DMA("sp", cst, cst_d[:, :], [], [cst_b])
        DMA("pool", cmat, cmat_d[:, :], [], [cmat_b])
        DMA("pool", wabd.rearrange("p a b -> p (a b)"), wabd_d[:, :], [], [wbd_b])
        DMA("pool", wibd.rearrange("p a b -> p (a b)"), wibd_d[:, :], [], [wbd_b])
        DMA("sp", wr_f, w_router_d.rearrange("(c p) e -> p c e", p=128), [], [wr_b])
        fw.op("dve", lambda e: e.memset(ones_bf, 1.0), [], [ones_b])
        fw.op("dve", lambda e: e.memset(cm, 0.0), [], [cm_b])
        fw.op("dve", lambda e: e.memset(carry, 0.0), [], carry_b)

        SM = {}
        so = 0
        for n_, w_ in [("cpar", 8), ("chalf", 8), ("bah", 8), ("bih", 8), ("negM", 1), ("mq", 1), ("mk", 1), ("t8", 8), ("lnh", 1)]:
            SM[n_] = small[:, so:so + w_]
            so += w_
        ACT(SM["t8"], P("lam"), AF.Exp, [prm_b], [small_b], scale=-1.0)
        ACT(SM["t8"], SM["t8"], AF.Ln, [small_b], [small_b], bias=1.0)
        TS(SM["cpar"], SM["t8"], -8.0, ALU.mult, [small_b], [small_b])
        TS(SM["chalf"], SM["t8"], -4.0, ALU.mult, [small_b], [small_b])
        TS(SM["bah"], P("ba"), 0.5, ALU.mult, [prm_b], [small_b])
        TS(SM["bih"], P("bi"), 0.5, ALU.mult, [prm_b], [small_b])
        fw.op("dve", lambda e: e.tensor_reduce(out=SM["mq"], in_=P("gq_bc"), axis=AX.X, op=ALU.max, apply_absolute_value=True), [prm_b], [small_b])
        fw.op("dve", lambda e: e.tensor_reduce(out=SM["mk"], in_=P("gk_bc"), axis=AX.X, op=ALU.max, apply_absolute_value=True), [prm_b], [small_b])
        TT(SM["negM"], SM["mq"], SM["mk"], ALU.mult, [small_b], [small_b])
        TS(SM["negM"], SM["negM"], -8.0, ALU.mult, [small_b], [small_b])
        cosc = rtab[:, 256:320]
        sinc = rtab[:, 320:384]
        TS(colt[:, 0, :], cosc, P("gq_col"), ALU.mult, [prm_b, cst_b], [colt_b])
        TS(colt[:, 1, :], sinc, P("gqp_col"), ALU.mult, [prm_b, cst_b], [colt_b])
        TS(colt[:, 2, :], cosc, P("gk_col"), ALU.mult, [prm_b, cst_b], [colt_b])
        TS(colt[:, 3, :], sinc, P("gkp_col"), ALU.mult, [prm_b, cst_b], [colt_b])
        for ch in range(4):
            for tap in range(4):
                TS(diagw[:, ch * 4 + tap, :], ident_bf, P("convw")[:, ch * 4 + tap:ch * 4 + tap + 1], ALU.mult, [prm_b, cmat_b], [diagw_b])

        _breg = {}

        def breg(e):
            if 'r' not in _breg:
                _breg['r'] = e.to_reg(NSLOT - 1)
            return _breg['r']

        scn = [0]

        def newsc():
            i = scn[0] % 64
            scn[0] += 1
            return sc[:, i:i + 1], sc_b[i]

        o = PH0
        kT = V(o, [128, S], BF16); o += 2 * S; kT_b = Buf()
        Vaug = V(o, [128, 64, 2, 65], BF16); o += 64 * 130 * 2; Vaug_b = Buf()
        qT = V(o, [128, 4, 2048], BF16); o += 16384; qT_b = Buf()
        o = (o + 63) // 64 * 64
        PHA = o
        Win = V(o, [128, 8, 1792], BF16); o += 8 * 1792 * 2; Win_b = Buf()
        xt = []
        for i in range(3):
            xt.append((V(o, [128, D]), Buf())); o += 4096
        xT = []
        for i in range(2):
            xT.append((V(o, [128, 8, 512], BF16), Buf())); o += 8192
        q_bf = V(o, [128, 512], BF16); o += 1024; qbf_b = Buf()
        sq_bf = V(o, [128, 512], BF16); o += 1024; sqbf_b = Buf()
        lnr = V(o, [128, 512]); o += 2048; lnr_b = Buf()
        TAq = V(o, [128, 8, 64]); o += 2048
        TBq = V(o, [128, 8, 64]); o += 2048; Tq_b = Buf()
        TAk = V(o, [128, 8, 64]); o += 2048
        TBk = V(o, [128, 8, 64]); o += 2048; Tk_b = Buf()
        t1 = V(o, [128, 512]); o += 2048; t1_b = Buf()
        t2 = V(o, [128, 512]); o += 2048; t2_b = Buf()
        vT_f = V(o, [128, 512]); o += 2048; vT_b = Buf()
        lst = []
        for i in range(2):
            lst.append((V(o, [128, 4, 512], BF16), Buf())); o += 4096
        ggst = []
        for i in range(2):
            ggst.append((V(o, [128, 4, 512]), Buf())); o += 8192
        wst = [(ggst[0][0].rearrange("p a b -> p (a b)")[:, 0:1792], ggst[0][1]),
               (ggst[1][0].rearrange("p a b -> p (a b)")[:, 0:1792], ggst[1][1])]

        for dc in range(8):
            ws, wsb = wst[dc % 2]
            DMA("sp", ws, w_in_d[dc * 128:(dc + 1) * 128, :], [], [wsb])
            g1 = P("g1c")[:, dc:dc + 1]
            TS(Win[:, dc, 0:512].rearrange("p (j h d) -> p h j d", j=4, h=2),
               ws[:, 0:512].rearrange("p (h j d) -> p h j d", h=2, j=4), g1, ALU.mult, [wsb, prm_b], [Win_b])
            TS(Win[:, dc, 512:1792], ws[:, 512:1792], g1, ALU.mult, [wsb, prm_b], [Win_b])
        fw.op("pool", lambda e: e.memset(Vaug[:, :, :, 64:65], 1.0), [], [Vaug_b])

        cosr = rtab[:, 0:128]
        sinr = rtab[:, 128:256]
        scr_lru_v = scr_lru.rearrange("c p t -> p c t")
        scr_gg_v = scr_gg.rearrange("c p t -> p c t")
        scrl_b = Buf()
        scrg_b = Buf()

        def rope(Pb, Pbb, TA, TB, Tb, outap, outb):
            CP("act", q_bf, Pb, [Pbb], [qbf_b])
            ACT(sq_bf, Pb, AF.Square, [Pbb], [sqbf_b])
            MM(pb[6], perm_bf, q_bf, True, True, [cmat_b, qbf_b], [pbb[6]])
            MM(pb[7], bones_bf, sq_bf, True, True, [cmat_b, sqbf_b], [pbb[7]])
            ACT(t2, pb[7], AF.Ln, [pbb[7]], [t2_b], scale=1.0 / 64, bias=1e-6)
            ACT(lnr, t2, AF.Exp, [t2_b], [lnr_b], scale=-0.5)
            if stop == "X1":
                CP("dve", t1, Pb, [Pbb, Tb], [t1_b])
            elif stop == "X2":
                TT(t1, Pb, lnr, ALU.mult, [Pbb, lnr_b], [t1_b])
            else:
                TT(t1, Pb, TA.rearrange("p a b -> p (a b)"), ALU.mult, [Pbb, Tb], [t1_b])
            TT(t2, pb[6], TB.rearrange("p a b -> p (a b)"), ALU.mult, [pbb[6], Tb], [t2_b])
            TT(t1, t1, t2, ALU.add, [t1_b, t2_b], [t1_b])
            TT(outap, t1, lnr, ALU.mult, [t1_b, lnr_b], [outb])

        def proj(acc_i, c0, xTi):
            xTa, xTbuf = xT[xTi]
            for dc in range(8):
                MM(pb[acc_i], Win[:, dc, c0:c0 + 128], xTa[:, dc, :], dc == 0, dc == 7, [Win_b, xTbuf], [pbb[acc_i]])

        acc_rr = [0]

        def nextacc():
            acc_rr[0] ^= 1
            return 4 + acc_rr[0]

        if stop == "S":
            fw.finish()
            return nc
        for s in range(4):
            for j in range(4):
                blk = s * 4 + j
                if stop == "A0" and blk == 1:
                    fw.finish()
                    return nc
                xTi = blk % 2
                xTa, xTbuf = xT[xTi]
                for t in range(4):
                    tile = blk * 4 + t
                    xa, xb = xt[tile % 3]
                    DMA("sp", xa, xs[tile * 128:(tile + 1) * 128, :], [], [xb])
                    ssq, ssqb = newsc()
                    ACT(junk, xa, AF.Square, [xb], [junk_b, ssqb], accum=ssq)
                    tmp, tmpb = newsc()
                    rs, rsb = newsc()
                    RSTD(rs, ssq, 1.0 / D, 1e-5, tmp, [ssqb], [tmpb, rsb])
                    TS(xa, xa, rs, ALU.mult, [xb, rsb], [xb])
                    for half in range(2):
                        bi = 2 * (tile % 2) + half
                        for q in range(4):
                            dc = half * 4 + q
                            TR(pb[bi][:, q * 128:(q + 1) * 128], xa[:, dc * 128:(dc + 1) * 128], identf, [xb, cst_b], [pbb[bi]])
                        CP("act" if half == 0 else "dve", xTa[:, half * 4:(half + 1) * 4, t * 128:(t + 1) * 128],
                           pb[bi].rearrange("p (a b) -> p a b", a=4), [pbb[bi]], [xTbuf])
                if stop == "A0a":
                    fw.finish()
                    return nc
                r0 = blk * 8
                TT(TAk, cosr[:, r0:r0 + 8].unsqueeze(2).to_broadcast([128, 8, 64]), colt[:, 2, :].unsqueeze(1).to_broadcast([128, 8, 64]),
                   ALU.mult, [cst_b, colt_b], [Tk_b])
                TT(TBk, sinr[:, r0:r0 + 8].unsqueeze(2).to_broadcast([128, 8, 64]), colt[:, 3, :].unsqueeze(1).to_broadcast([128, 8, 64]),
                   ALU.mult, [cst_b, colt_b], [Tk_b])
                a = nextacc()
                proj(a, 512, xTi)
                rope(pb[a], pbb[a], TAk, TBk, Tk_b, kT[:, blk * 512:(blk + 1) * 512], kT_b)
                if stop in ("A0b", "X1", "X2"):
                    fw.finish()
                    return nc
                a = nextacc()
                proj(a, 640, xTi)
                CP("act", vT_f, pb[a], [pbb[a]], [vT_b])
                for t in range(4):
                    TR(pb[6][:, t * 128:(t + 1) * 128], vT_f[:, t * 128:(t + 1) * 128], identf, [vT_b, cst_b], [pbb[6]])
                CP("dve", Vaug[:, blk * 4:(blk + 1) * 4, :, 0:64], pb[6].rearrange("p (t h d) -> p t h d", t=4, h=2), [pbb[6]], [Vaug_b])
                if stop == "A0c":
                    fw.finish()
                    return nc
                la, lb = lst[blk % 2]
                for cc in range(4):
                    a = nextacc()
                    proj(a, 768 + cc * 128, xTi)
                    CP("act" if cc % 2 == 0 else "dve", la[:, cc, :], pb[a], [pbb[a]], [lb])
                DMA("sp", scr_lru_v[:, :, 2 + blk * 512:2 + (blk + 1) * 512], la, [lb], [scrl_b])
                if blk == 15:
                    DMA("sp", scr_lru_v[:, :, 0:2], la[:, :, 510:512], [lb], [scrl_b], slow=True)
                if blk == 0:
                    DMA("sp", scr_lru_v[:, :, S + 2:S + 3], la[:, :, 0:1], [lb], [scrl_b], slow=True)
                if stop == "A0d":
                    fw.finish()
                    return nc
                if s == 0:
                    TT(TAq, cosr[:, r0:r0 + 8].unsqueeze(2).to_broadcast([128, 8, 64]), colt[:, 0, :].unsqueeze(1).to_broadcast([128, 8, 64]),
                       ALU.mult, [cst_b, colt_b], [Tq_b])
                    TT(TBq, sinr[:, r0:r0 + 8].unsqueeze(2).to_broadcast([128, 8, 64]), colt[:, 1, :].unsqueeze(1).to_broadcast([128, 8, 64]),
                       ALU.mult, [cst_b, colt_b], [Tq_b])
                    for pr in range(4):
                        a = nextacc()
                        proj(a, pr * 128, xTi)
                        rope(pb[a], pbb[a], TAq, TBq, Tq_b, qT[:, pr, j * 512:(j + 1) * 512], qT_b)
                    ga, gb = ggst[blk % 2]
                    for cc in range(4):
                        a = nextacc()
                        proj(a, 1280 + cc * 128, xTi)
                        ACT(t1, pb[a], AF.Square, [pbb[a]], [t1_b])
                        CP("act", t2, pb[a], [pbb[a]], [t2_b])
                        TS(t1, t1, 0.044715, ALU.mult, [t1_b], [t1_b], s2=1.0, op1=ALU.add)
                        TT(t1, t1, t2, ALU.mult, [t1_b, t2_b], [t1_b])
                        ACT(lnr, t1, AF.Sigmoid, [t1_b], [lnr_b], scale=2.0 * math.sqrt(2.0 / math.pi))
                        TT(ga[:, cc, :], lnr, t2, ALU.mult, [lnr_b, t2_b], [gb])
                    DMA("sp", scr_gg_v[:, :, j * 512:(j + 1) * 512], ga, [gb], [scrg_b])

        if dbg:
            DMA("sp", dbg_out["d_kT"][:, :], kT, [kT_b], [Buf()])
            DMA("sp", dbg_out["d_qT"][:, :], qT.rearrange("p a b -> p (a b)"), [qT_b], [Buf()])
            DMA("sp", dbg_out["d_V"][:, :], Vaug.rearrange("p a b c -> p (a b c)"), [Vaug_b], [Buf()])
        if stop == "A":
            fw.finish()
            return nc
        fw.barrier()

        o = PHA
        attn_raw = V(o, [128, 16, 512]); o += 32768; attn_b = Buf()
        pT = []
        for i in range(3):
            pT.append((V(o, [128, 512], BF16), Buf())); o += 1024
        rc = V(o, [128, 4]); o += 16; rc_b = Buf()
        o = (o + 63) // 64 * 64
        aT_off = o
        aT_bf = V(o, [128, 16, 4, 128], BF16); o += 16384; aT_b = Buf()
        negM = SM["negM"]
        it = 0
        for qb in range(4):
            for h in range(8):
                pr, kv = h % 4, h // 4
                rows = slice(kv * 64, (kv + 1) * 64)
                oi = 3 + (it % 2)
                it += 1
                for kc in range(64):
                    si = kc % 3
                    pa, pab = pT[si]
                    MM(pb[si], kT[rows, kc * 128:(kc + 1) * 128], qT[rows, pr, qb * 512:(qb + 1) * 512], True, True, [kT_b, qT_b], [pbb[si]])
                    ACT(pa, pb[si], AF.Exp, [pbb[si], small_b], [pab], scale=0.125, bias=negM)
                    for qt in range(4):
                        MM(pb[oi][:, qt * 65:(qt + 1) * 65], pa[:, qt * 128:(qt + 1) * 128], Vaug[:, kc, kv, :],
                           kc == 0 and qt == 0, kc == 63, [pab, Vaug_b], [pbb[oi]], skip=True)
                ov = pb[oi][:, 0:260].rearrange("p (a b) -> p a b", a=4)
                fw.op("dve", lambda e, ov=ov: e.reciprocal(out=rc, in_=ov[:, :, 64]), [pbb[oi]], [rc_b])
                for qt in range(4):
                    TS(attn_raw[:, qb * 4 + qt, h * 64:(h + 1) * 64], ov[:, qt, 0:64], rc[:, qt:qt + 1], ALU.mult, [pbb[oi], rc_b], [attn_b])
            for qt in range(4):
                tile = qb * 4 + qt
                ssq, ssqb = newsc()
                ACT(junk[:, 0:512], attn_raw[:, tile, :], AF.Square, [attn_b], [junk_b, ssqb], accum=ssq)
                tmp, tmpb = newsc()
                RSTD(rstd_a[:, tile:tile + 1], ssq, 1.0 / 512, 1e-5, tmp, [ssqb], [tmpb, rstda_b])
                for cc in range(4):
                    TR(pb[5][:, cc * 128:(cc + 1) * 128], attn_raw[:, tile, cc * 128:(cc + 1) * 128], identf, [attn_b, cst_b], [pbb[5]])
                CP("dve", aT_bf[:, tile, :, :], pb[5].rearrange("p (a b) -> p a b", a=4), [pbb[5]], [aT_b])
        if dbg:
            DMA("sp", dbg_out["d_attn"][:, :], attn_raw.rearrange("p a b -> p (a b)"), [attn_b], [Buf()])
        if stop == "B":
            fw.finish()
            return nc
        fw.barrier()

        o = PH0
        lru_acc = V(o, [128, 4, 2048]); o += 32768; lacc_b = [Buf() for _ in range(4)]
        lruT_off = o
        lruT_bf = V(o, [128, 4, 2048], BF16); o += 16384; lruT_b = Buf()
        regs = [(o, aT_off), (aT_off + 16384, SB_BYTES)]
        ptr = [regs[0][0], 0]

        def lalloc(nbytes):
            if ptr[0] + nbytes > regs[ptr[1]][1]:
                ptr[1] += 1
                ptr[0] = regs[ptr[1]][0]
            r = ptr[0]
            ptr[0] += (nbytes + 63) // 64 * 64
            assert ptr[0] <= regs[ptr[1]][1]
            return r

        xl = [(V(lalloc(4112), [128, 2056], BF16), Buf()) for _ in range(2)]
        xc_f = V(lalloc(8192), [128, 2048]); xcf_b = Buf()
        xc_b = V(lalloc(4096), [128, 2048], BF16); xcb_b = Buf()
        th_a = V(lalloc(8192), [128, 2048]); tha_b = Buf()
        th_i = V(lalloc(8192), [128, 2048]); thi_b = Buf()
        a_t = V(lalloc(8192), [128, 2048]); at_b = Buf()
        tmp_t = V(lalloc(8192), [128, 2048]); tmpt_b = Buf()
        bb_t = V(lalloc(8192), [128, 2048]); bbt_b = Buf()
        hh_t = V(lalloc(8192), [128, 2048]); hht_b = Buf()
        gg_t = xc_f; ggt_b = xcf_b
        LN_HALF = math.log(0.5)
        n_unit = 0
        for d_ in range(2):
            order = [1, 2, 3, 0] if d_ == 0 else [3, 2, 1, 0]
            for slot in order:
                for ch in range(4):
                    xla, xlb = xl[n_unit % 2]
                    n_unit += 1
                    DMA("sp", xla[:, 0:2051], scr_lru[ch, :, slot * 2048:slot * 2048 + 2051], [scrl_b], [xlb])
                    TS(xla[:, 0:2], xla[:, 0:2], msk[:, slot:slot + 1], ALU.mult, [xlb, cst_b], [xlb])
                    TS(xla[:, 2050:2051], xla[:, 2050:2051], msk[:, 4 + slot:5 + slot], ALU.mult, [xlb, cst_b], [xlb])
                    pidx = d_ * 4 + ch
                    for jb in range(4):
                        cs = slice(jb * 512, (jb + 1) * 512)
                        for tap in range(4):
                            MM(pb[0], diagw[:, ch * 4 + tap, :], xla[:, jb * 512 + tap:jb * 512 + tap + 512], tap == 0, tap == 3,
                               [diagw_b, xlb], [pbb[0]])
                        ACT(xc_f[:, cs], pb[0], AF.Identity, [pbb[0], prm_b], [xcf_b], bias=P("convb")[:, ch:ch + 1])
                        TS(xc_b[:, cs], pb[0], P("convb")[:, ch:ch + 1], ALU.add, [pbb[0], prm_b], [xcb_b])
                        MM(pb[1], wabd[:, pidx, :], xc_b[:, cs], True, True, [wbd_b, xcb_b], [pbb[1]])
                        MM(pb[2], wibd[:, pidx, :], xc_b[:, cs], True, True, [wbd_b, xcb_b], [pbb[2]])
                        ACT(th_a[:, cs], pb[1], AF.Tanh, [pbb[1], small_b], [tha_b], scale=0.5, bias=SM["bah"][:, pidx:pidx + 1])
                        ACT(th_i[:, cs], pb[2], AF.Tanh, [pbb[2], small_b], [thi_b], scale=0.5, bias=SM["bih"][:, pidx:pidx + 1])
                    ch_ = SM["chalf"][:, pidx:pidx + 1]
                    c_ = SM["cpar"][:, pidx:pidx + 1]
                    ACT(a_t, th_a, AF.Exp, [tha_b, small_b], [at_b], scale=ch_, bias=ch_)
                    ACT(tmp_t, th_a, AF.Exp, [tha_b, small_b], [tmpt_b], scale=c_, bias=c_)
                    ACT(bb_t, tmp_t, AF.Ln, [tmpt_b], [bbt_b], scale=-1.0, bias=1.0)
                    ACT(tmp_t, bb_t, AF.Exp, [bbt_b], [tmpt_b], scale=0.5, bias=LN_HALF)
                    TT(tmp_t, tmp_t, xc_f, ALU.mult, [tmpt_b, xcf_b], [tmpt_b])
                    STT(bb_t, th_i, 1.0, tmp_t, ALU.add, ALU.mult, [thi_b, tmpt_b], [bbt_b])
                    cidx = d_ * 4 + ch
                    cb = carry_b[cidx]
                    ini, inib = newsc()
                    mcol = msk[:, slot:slot + 1] if d_ == 0 else msk[:, 4 + slot:5 + slot]
                    TS(ini, carry[:, cidx:cidx + 1], mcol, ALU.mult, [cb, cst_b], [inib])
                    last = slot == 0
                    if last and d_ == 0:
                        dst, dstb = lru_acc[:, ch, :], lacc_b[ch]
                    else:
                        dst, dstb = hh_t, hht_b
                    if d_ == 0:
                        fw.op("dve", lambda e, dst=dst, ini=ini: e.tensor_tensor_scan(out=dst, data0=a_t, data1=bb_t, initial=ini,
                                                                                      op0=ALU.mult, op1=ALU.add),
                              [at_b, bbt_b, inib], [dstb])
                        CP("dve", carry[:, cidx:cidx + 1], dst[:, 2047:2048], [dstb], [cb])
                    else:
                        fw.op("dve", lambda e, dst=dst, ini=ini: e.tensor_tensor_scan(out=dst[:, ::-1], data0=a_t[:, ::-1], data1=bb_t[:, ::-1],
                                                                                      initial=ini, op0=ALU.mult, op1=ALU.add),
                              [at_b, bbt_b, inib], [dstb])
                        CP("dve", carry[:, cidx:cidx + 1], dst[:, 0:1], [dstb], [cb])
                    if last and d_ == 1:
                        TT(lru_acc[:, ch, :], lru_acc[:, ch, :], hh_t, ALU.add, [lacc_b[ch], hht_b], [lacc_b[ch]], eng="pool")
                        DMA("sp", gg_t, scr_gg[ch, :, :], [scrg_b], [ggt_b])
                        TT(lruT_bf[:, ch, :], lru_acc[:, ch, :], gg_t, ALU.mult, [lacc_b[ch], ggt_b], [lruT_b], eng="pool")
        if dbg:
            DMA("sp", dbg_out["d_lru"][:, :], lruT_bf.rearrange("p a b -> p (a b)"), [lruT_b], [Buf()])
        if stop == "C":
            fw.finish()
            return nc
        fw.barrier()

        o = PH0
        wout = V(o, [128, 8, D], BF16); o += 16384; wout_b = Buf()
        assert o <= lruT_off
        regs = [(lruT_off + 16384, aT_off), (aT_off + 16384, SB_BYTES)]
        ptr = [regs[0][0], 0]
        xq = [(V(lalloc(4096), [128, D]), Buf()) for _ in range(2)]
        xn2 = [(V(lalloc(4096), [128, D]), Buf()) for _ in range(2)]
        PB2 = []
        for _par in range(2):
            dct = {}
            dct["xn2T"] = (V(lalloc(4096), [128, 8, 128]), Buf())
            dct["sqt"] = (V(lalloc(1024), [128, 4, 128], BF16), Buf())
            for nm_ in ["lg", "mask", "ex", "g32", "sv", "oh"]:
                dct[nm_] = (V(lalloc(128), [128, NE]), Buf())
            for nm_ in ["v8", "s8", "e8"]:
                dct[nm_] = (V(lalloc(64), [128, 8]), Buf())
            PB2.append(dct)
        wos = [(V(lalloc(4096), [128, D]), Buf()) for _ in range(2)]
        for dc in range(8):
            ws, wsb = wos[dc % 2]
            DMA("sp", ws, w_out_d[dc * 128:(dc + 1) * 128, :], [], [wsb])
            gcol = P("gao")[:, dc:dc + 1] if dc < 4 else P("glo")[:, dc - 4:dc - 3]
            TS(wout[:, dc, :], ws, gcol, ALU.mult, [wsb, prm_b], [wout_b])
        scrx1_b = Buf()
        scrxd_b = Buf()
        ustrict = Cc("ustrict")
        onesf = Cc("onesf")
        iota1 = Cc("iota1")
        slot1 = Cc("slot1")
        for i in range(16):
            xa, xb = xq[i % 2]
            na, nb = xn2[i % 2]
            dct = PB2[i % 2]
            xn2T, xn2T_b = dct["xn2T"]
            sqt, sqt_b = dct["sqt"]
            lg, lg_b = dct["lg"]
            mask, mask_b = dct["mask"]
            ex, ex_b = dct["ex"]
            g32, g32_b = dct["g32"]
            sv, sv_b = dct["sv"]
            oh, oh_b = dct["oh"]
            v8, v8_b = dct["v8"]
            s8, s8_b = dct["s8"]
            e8, e8_b = dct["e8"]
            DMA("sp", xa, xs[i * 128:(i + 1) * 128, :], [], [xb])
            for half in range(2):
                hs = slice(half * 512, (half + 1) * 512)
                for cc in range(4):
                    MM(pb[half], aT_bf[:, i, cc, :], wout[:, cc, hs], cc == 0, cc == 3, [aT_b, wout_b], [pbb[half]])
                for cc in range(4):
                    MM(pb[2 + half], lruT_bf[:, cc, i * 128:(i + 1) * 128], wout[:, 4 + cc, hs], cc == 0, cc == 3, [lruT_b, wout_b], [pbb[2 + half]])
            ACT(sqt, lruT_bf[:, :, i * 128:(i + 1) * 128], AF.Square, [lruT_b], [sqt_b])
            for cc in range(4):
                MM(pb[4][:, 0:2], sqt[:, cc, :], ones_bf, cc == 0, cc == 3, [sqt_b, ones_b], [pbb[4]])
            tmp, tmpb = newsc()
            rl, rlb = newsc()
            RSTD(rl, pb[4][:, 0:1], 1.0 / 512, 1e-5, tmp, [pbb[4]], [tmpb, rlb])
            for half in range(2):
                hs = slice(half * 512, (half + 1) * 512)
                STT(xa[:, hs], pb[half], rstd_a[:, i:i + 1], xa[:, hs], ALU.mult, ALU.add, [pbb[half], rstda_b, xb], [xb])
                STT(xa[:, hs], pb[2 + half], rl, xa[:, hs], ALU.mult, ALU.add, [pbb[2 + half], rlb, xb], [xb])
            DMA("sp", scr_x1[i * 128:(i + 1) * 128, :], xa, [xb], [scrx1_b])
            if dbg:
                DMA("sp", dbg_out["d_x1"][i * 128:(i + 1) * 128, :], xa, [xb], [Buf()])
            ssq, ssqb = newsc()
            ACT(junk, xa, AF.Square, [xb], [junk_b, ssqb], accum=ssq)
            tmp, tmpb = newsc()
            r2, r2b = newsc()
            RSTD(r2, ssq, 1.0 / D, 1e-5, tmp, [ssqb], [tmpb, r2b])
            STT(na, xa, r2, P("g2"), ALU.mult, ALU.mult, [xb, r2b, prm_b], [nb])
            for half in range(2):
                for q in range(4):
                    dc = half * 4 + q
                    TR(pb[5 + half][:, q * 128:(q + 1) * 128], na[:, dc * 128:(dc + 1) * 128], identf, [nb, cst_b], [pbb[5 + half]])
                CP("act", xn2T[:, half * 4:(half + 1) * 4, :], pb[5 + half].rearrange("p (a b) -> p a b", a=4), [pbb[5 + half]], [xn2T_b])
            for dc in range(8):
                MM(pb[7][:, 0:NE], xn2T[:, dc, :], wr_f[:, dc, :], dc == 0, dc == 7, [xn2T_b, wr_b], [pbb[7]])
            TT(lg, pb[7][:, 0:NE], P("brt"), ALU.add, [pbb[7], prm_b], [lg_b])
            fw.op("dve", lambda e, v8=v8, lg=lg: e.max(out=v8, in_=lg), [lg_b], [v8_b])
            TS(mask, lg, v8[:, 3:4], ALU.is_ge, [lg_b, v8_b], [mask_b])
            nv, nvb = newsc()
            TS(nv, v8[:, 0:1], -1.0, ALU.mult, [v8_b], [nvb])
            ACT(ex, lg, AF.Exp, [lg_b, nvb], [ex_b], bias=nv)
            sm_, smb = newsc()
            STT(ex, ex, 1.0, mask, ALU.mult, ALU.mult, [ex_b, mask_b], [ex_b, smb], accum=sm_)
            rs_, rsb = newsc()
            fw.op("dve", lambda e, rs_=rs_, sm_=sm_: e.reciprocal(out=rs_, in_=sm_), [smb], [rsb])
            TS(g32, ex, rs_, ALU.mult, [ex_b, rsb], [g32_b])
            MM(pb[4][:, 32:64], ustrict, mask, True, False, [cst_b, mask_b], [pbb[4]])
            MM(pb[4][:, 32:64], onesf, cm, False, True, [cst_b, cm_b], [pbb[4]])
            TT(sv, pb[4][:, 32:64], slot1, ALU.add, [pbb[4], cst_b], [sv_b])
            TT(sv, sv, mask, ALU.mult, [sv_b, mask_b], [sv_b])
            TT(cm, cm, mask, ALU.add, [cm_b, mask_b], [cm_b])
            fw.op("dve", lambda e, s8=s8, sv=sv: e.max(out=s8, in_=sv), [sv_b], [s8_b])
            TS(dest4[:, i, :], s8[:, 0:4], -1.0, ALU.add, [s8_b], [dest4_b])
            TT(sv, mask, iota1, ALU.mult, [mask_b, cst_b, sv_b], [sv_b])
            fw.op("dve", lambda e, e8=e8, sv=sv: e.max(out=e8, in_=sv), [sv_b], [e8_b])
            for k in range(4):
                TS(oh, iota1, e8[:, k:k + 1], ALU.is_equal, [cst_b, e8_b], [oh_b])
                STT(oh, oh, 1.0, g32, ALU.mult, ALU.mult, [oh_b, g32_b], [oh_b, gate4_b], accum=gate4[:, i, k:k + 1])
            for k in range(4):
                fw.dma("pool", lambda e, na=na, i=i, k=k: e.indirect_dma_start(
                    out=scr_xd[:, :], out_offset=bass.IndirectOffsetOnAxis(ap=dest4[:, i, k:k + 1], axis=0),
                    in_=na, in_offset=None, bounds_check=breg(e), oob_is_err=False), [nb, dest4_b], [scrxd_b])
        if stop == "D":
            fw.finish()
            return nc
        fw.barrier()

        o = PH0
        wring = []
        for i in range(6):
            wring.append((V(o, [128, 8, D], BF16), Buf())); o += 16384
        xe_h = [(V(o + hh_ * 8192, [128, 2, D]), Buf()) for hh_ in range(2)]
        o += 16384
        xeT = V(o, [128, 8, SUBC], BF16); o += 8 * SUBC * 2; xeT_b = Buf()
        hT = V(o, [128, 8, SUBC], BF16); o += 8 * SUBC * 2; hT_b = Buf()
        bdb = []
        for i in range(1):
            bdb.append((V(o, [128, D]), Buf())); o += 4096
        yst = []
        for i in range(2):
            yst.append((V(o, [128, D]), Buf())); o += 4096
        gc = V(o, [128, SUBC]); o += SUBC * 4; gc_b = Buf()
        sg = V(o, [128, SUBC]); o += SUBC * 4; sg_b = Buf()
        uc = V(o, [128, SUBC]); o += SUBC * 4; uc_b = Buf()
        assert o <= SB_BYTES, o
        scry_b = Buf()
        bg = P("bg")
        bu = P("bu")
        ny = 0
        for ex_i in range(NE):
            wg, wgb = wring[(ex_i % 2) * 3 + 0]
            wu, wub = wring[(ex_i % 2) * 3 + 1]
            wd, wdb = wring[(ex_i % 2) * 3 + 2]
            DMA("pool", wg, w_gate_d[ex_i].rearrange("(c p) f -> p c f", p=128), [], [wgb])
            DMA("pool", wu, w_up_d[ex_i].rearrange("(c p) f -> p c f", p=128), [], [wub])
            DMA("pool", wd, w_down_d[ex_i].rearrange("(c p) f -> p c f", p=128), [], [wdb])
            bda, bdbb = bdb[0]
            DMA("sp", bda, b_down_d[ex_i:ex_i + 1, :].partition_broadcast(128), [], [bdbb])
            for sbk in range(CAP // SUBC):
                base = ex_i * CAP + sbk * SUBC
                for hh_ in range(2):
                    DMA("sp", xe_h[hh_][0], scr_xd[base + hh_ * 256:base + (hh_ + 1) * 256, :].rearrange("(t p) d -> p t d", p=128),
                        [scrxd_b], [xe_h[hh_][1]])
                for t in range(4):
                    xe_f, xef_b = xe_h[t // 2]
                    for half in range(2):
                        bi = half
                        for q in range(4):
                            dc = half * 4 + q
                            TR(pb[bi][:, q * 128:(q + 1) * 128], xe_f[:, t % 2, dc * 128:(dc + 1) * 128], identf, [xef_b, cst_b], [pbb[bi]])
                        CP("act", xeT[:, half * 4:(half + 1) * 4, t * 128:(t + 1) * 128], pb[bi].rearrange("p (a b) -> p a b", a=4), [pbb[bi]], [xeT_b])
                for fc in range(8):
                    fs = slice(fc * 128, (fc + 1) * 128)
                    pg, pu = 2 + 2 * (fc % 2), 3 + 2 * (fc % 2)
                    for dc in range(8):
                        MM(pb[pg], wg[:, dc, fs], xeT[:, dc, :], dc == 0, dc == 7, [wgb, xeT_b], [pbb[pg]])
                    for dc in range(8):
                        MM(pb[pu], wu[:, dc, fs], xeT[:, dc, :], dc == 0, dc == 7, [wub, xeT_b], [pbb[pu]])
                    bgc = bg[:, ex_i * 8 + fc:ex_i * 8 + fc + 1]
                    buc = bu[:, ex_i * 8 + fc:ex_i * 8 + fc + 1]
                    TS(gc, pb[pg], bgc, ALU.add, [pbb[pg], prm_b], [gc_b], s2=7.0, op1=ALU.min)
                    ACT(sg, gc, AF.Sigmoid, [gc_b], [sg_b], scale=1.702)
                    TS(uc, pb[pu], buc, ALU.add, [pbb[pu], prm_b], [uc_b], s2=7.0, op1=ALU.min)
                    TS(uc, uc, -7.0, ALU.max, [uc_b], [uc_b], s2=1.0, op1=ALU.add)
                    TT(gc, gc, sg, ALU.mult, [gc_b, sg_b], [gc_b])
                    TT(hT[:, fc, :], uc, gc, ALU.mult, [uc_b, gc_b], [hT_b])
                for t in range(4):
                    ya, yb = yst[ny % 2]
                    ny += 1
                    for half in range(2):
                        hs = slice(half * 512, (half + 1) * 512)
                        pi = 6 + half
                        for fc in range(8):
                            MM(pb[pi], hT[:, fc, t * 128:(t + 1) * 128], wd[:, fc, hs], fc == 0, fc == 7, [hT_b, wdb], [pbb[pi]])
                        TT(ya[:, hs], pb[pi], bda[:, hs], ALU.add, [pbb[pi], bdbb], [yb])
                    DMA("act", scr_y[base + t * 128:base + (t + 1) * 128, :], ya, [yb], [scry_b])
        if stop == "E":
            fw.finish()
            return nc
        fw.barrier()

        o = PH0
        acc = [(V(o + i * 4096, [128, D]), Buf()) for i in range(2)]
        o += 8192
        yk = [(V(o + i * 4096, [128, D]), Buf()) for i in range(4)]
        o += 16384
        ot = [(V(o + i * 4096, [128, D]), Buf()) for i in range(2)]
        yout_b = Buf()
        for i in range(16):
            aa, ab = acc[i % 2]
            DMA("sp", aa, scr_x1[i * 128:(i + 1) * 128, :], [scrx1_b], [ab])
            for k in range(4):
                ya, yb = yk[k]
                fw.dma("pool", lambda e, ya=ya, i=i, k=k: e.indirect_dma_start(
                    out=ya, out_offset=None, in_=scr_y[:, :],
                    in_offset=bass.IndirectOffsetOnAxis(ap=dest4[:, i, k:k + 1], axis=0), bounds_check=breg(e), oob_is_err=False),
                    [scry_b, dest4_b], [yb])
                STT(aa, ya, gate4[:, i, k:k + 1], aa, ALU.mult, ALU.add, [yb, gate4_b, ab], [ab])
            ssq, ssqb = newsc()
            ACT(junk, aa, AF.Square, [ab], [junk_b, ssqb], accum=ssq)
            tmp, tmpb = newsc()
            rf, rfb = newsc()
            RSTD(rf, ssq, 1.0 / D, 1e-5, tmp, [ssqb], [tmpb, rfb])
            oa, ob = ot[i % 2]
            STT(oa, aa, rf, P("gf"), ALU.mult, ALU.mult, [ab, rfb, prm_b], [ob])
            DMA("sp", y_d[i * 128:(i + 1) * 128, :], oa, [ob], [yout_b])
        fw.finish()
    return nc


def _rope_tables(c):
    invf = (10000.0 ** (-np.arange(16, dtype=np.float64) / 16.0))
    tab = np.ones((128, 384), np.float64)
    R = np.arange(128)
    rows = ((c + R // 32) % 4) * 32 + R % 32
    cols = np.arange(64)
    for p in range(128):
        d = p % 64
        sgn = -1.0 if (d % 32) < 16 else 1.0
        f = d % 16
        if d < 32:
            tab[p, 0:128] = np.cos(rows * invf[f])
            tab[p, 128:256] = sgn * np.sin(rows * invf[f])
            tab[p, 256:320] = 1.0
            tab[p, 320:384] = 1.0
        else:
            tab[p, 0:128] = 1.0
            tab[p, 128:256] = 1.0
            tab[p, 256:320] = np.cos(cols * invf[f])
            tab[p, 320:384] = sgn * np.sin(cols * invf[f])
    return tab.astype(np.float32)


def _consts(c):
    cst = np.zeros((128, NCST), np.float32)
    a, w = CST["identf"]; cst[:, a:a + w] = np.eye(128, dtype=np.float32)
    a, w = CST["ustrict"]; cst[:, a:a + w] = np.triu(np.ones((128, 128), np.float32), 1)
    a, w = CST["onesf"]; cst[:, a:a + w] = 1.0
    a, w = CST["rtab"]; cst[:, a:a + w] = _rope_tables(c)
    a, w = CST["msk"]
    for s in range(4):
        cst[:, a + s] = 0.0 if (c + s) % 4 == 0 else 1.0
        cst[:, a + 4 + s] = 0.0 if (c + s) % 4 == 3 else 1.0
    a, w = CST["iota1"]; cst[:, a:a + w] = np.arange(1, 33, dtype=np.float32)[None, :]
    a, w = CST["slot1"]; cst[:, a:a + w] = (np.arange(32, dtype=np.float32) * CAP + 1.0)[None, :]
    cmat = np.zeros((128, 384), np.float32)
    cmat[:, 0:128] = np.eye(128)
    for m in range(128):
        d = m % 64
        base = m - d
        blk = d - d % 32
        i = d % 32
        pi = i + 16 if i < 16 else i - 16
        cmat[base + blk + pi, 128 + m] = 1.0
    cmat[0:64, 256:320] = 1.0
    cmat[64:128, 320:384] = 1.0
    return cst, cmat


def _partner64():
    d = np.arange(64)
    i = d % 32
    return d - i + np.where(i < 16, i + 16, i - 16)


def _prm(inp):
    prm = np.zeros((128, NPRM), np.float32)

    def put(name, arr):
        a, w = PRM[name]
        prm[:, a:a + w] = np.asarray(arr, np.float32).reshape(128, w)

    col = lambda v, n: np.ascontiguousarray(np.asarray(v).reshape(n, 128).T)
    gq = np.asarray(inp["q_norm_g"]).reshape(64)
    gk = np.asarray(inp["k_norm_g"]).reshape(64)
    pp = _partner64()
    put("g1c", col(inp["norm1_g"].reshape(-1), 8))
    put("gq_bc", np.broadcast_to(gq, (128, 64)))
    put("gk_bc", np.broadcast_to(gk, (128, 64)))
    d64 = np.arange(128) % 64
    put("gq_col", gq[d64]); put("gqp_col", gq[pp[d64]]); put("gk_col", gk[d64]); put("gkp_col", gk[pp[d64]])
    cw = np.asarray(inp["conv_w"]).reshape(4, 4, 128)
    put("convw", np.transpose(cw, (2, 1, 0)).reshape(128, 16))
    put("convb", col(inp["conv_b"].reshape(-1), 4))
    for nm, key in [("ba", "lru_ba"), ("bi", "lru_bi"), ("lam", "lru_lam")]:
        v = np.asarray(inp[key]).reshape(2, 4, 128)
        put(nm, np.transpose(v, (2, 0, 1)).reshape(128, 8))
    put("gao", col(inp["attn_out_g"].reshape(-1), 4))
    put("glo", col(inp["lru_out_g"].reshape(-1), 4))
    put("brt", np.broadcast_to(np.asarray(inp["b_router"]).reshape(32), (128, 32)))
    put("g2", np.broadcast_to(np.asarray(inp["norm2_g"]).reshape(1024), (128, 1024)))
    put("gf", np.broadcast_to(np.asarray(inp["final_g"]).reshape(1024), (128, 1024)))
    for nm, key in [("bg", "b_gate"), ("bu", "b_up")]:
        v = np.asarray(inp[key]).reshape(32, 8, 128)
        put(nm, np.transpose(v, (2, 0, 1)).reshape(128, 256))
    return prm


def _blockdiag(w):
    w = np.asarray(w).reshape(2, 4, 2, 64, 64)
    out = np.zeros((128, 2, 4, 128), np.float32)
    for d in range(2):
        for ch in range(4):
            for b in range(2):
                out[b * 64:(b + 1) * 64, d, ch, b * 64:(b + 1) * 64] = w[d, ch, b]
    return out.reshape(128, 1024)


def make_in_maps(inp):
    x = np.asarray(inp["x"], np.float32)
    shared = {
        "prm": _prm(inp),
        "w_in": np.ascontiguousarray(np.asarray(inp["w_in"], np.float32).reshape(D, 1792)),
        "wabd": _blockdiag(inp["lru_wa"]),
        "wibd": _blockdiag(inp["lru_wi"]),
        "w_out": np.ascontiguousarray(np.asarray(inp["w_out"], np.float32).reshape(D, D)),
        "w_router": np.ascontiguousarray(np.asarray(inp["w_router"], np.float32).reshape(D, NE)),
        "w_gate": np.ascontiguousarray(np.asarray(inp["w_gate"], np.float32).reshape(NE, D, D)),
        "w_up": np.ascontiguousarray(np.asarray(inp["w_up"], np.float32).reshape(NE, D, D)),
        "w_down": np.ascontiguousarray(np.asarray(inp["w_down"], np.float32).reshape(NE, D, D)),
        "b_down": np.ascontiguousarray(np.asarray(inp["b_down"], np.float32).reshape(NE, D)),
    }
    maps = []
    for r in range(8):
        b, c = r // 4, r % 4
        order = [(c + s) % 4 for s in range(4)]
        xsr = np.concatenate([x[b, k * 2048:(k + 1) * 2048] for k in order], axis=0)
        cst, cmat = _consts(c)
        m = dict(shared)
        m["xs"] = np.ascontiguousarray(xsr)
        m["cst"] = cst
        m["cmat"] = cmat
        maps.append(m)
    return maps


def kernel(**inputs):
    nc = build_nc()
    in_maps = make_in_maps(inputs)
    res = run_bass_kernel_spmd(nc, in_maps, core_ids=list(range(8)))
    out = np.zeros((2, S, D), np.float32)
    for r in range(8):
        b, c = r // 4, r % 4
        out[b, c * 2048:(c + 1) * 2048] = np.asarray(res.results[r]["y"], np.float32)
    return out
```

```python
import math
import numpy as np
from contextlib import ExitStack
import concourse.bass as bass
import concourse.mybir as mybir
from concourse.alu_op_type import AluOpType as ALU
from concourse.bass_utils import run_bass_kernel_spmd

F32 = mybir.dt.float32
BF16 = mybir.dt.bfloat16
I32 = mybir.dt.int32
AF = mybir.ActivationFunctionType
AX = mybir.AxisListType

S = 8192
D = 1024
NE = 32
CAP = 1024
SUBC = 512
NSLOT = NE * CAP
SB_BYTES = 180224


class Buf:
    __slots__ = ("w", "r", "x")

    def __init__(self, x=False):
        self.w = None
        self.r = []
        self.x = x


class FW:
    def __init__(self, nc, ctx, n_dma_sems=40):
        self.nc = nc
        self.eng = {"pe": nc.tensor, "dve": nc.vector, "act": nc.scalar, "pool": nc.gpsimd, "sp": nc.sync}
        self.psem, self.cnt, self.seen = {}, {}, {}
        for e in self.eng:
            self.psem[e] = ctx.enter_context(nc.semaphore("ps_" + e))
            self.cnt[e] = 0
            self.seen[e] = {}
        self.dsems = [ctx.enter_context(nc.semaphore("dq%d" % i)) for i in range(n_dma_sems)]
        self.dval = [0] * n_dma_sems
        self.dnext = 0
        self.nsw = 16
        self.swnext = 0
        self.prog = {e: [] for e in self.eng}
        self.nops = 0
        self.limit = None
        self.log = []

    def _wait(self, e, ev):
        if ev is None:
            return
        sem, val = ev
        k = id(sem)
        if self.seen[e].get(k, 0) >= val:
            return
        self.prog[e].append(("w", sem, val))
        self.seen[e][k] = val

    def _deps(self, e, reads, writes):
        own = id(self.psem[e])
        for b in reads:
            ev = b.w
            if ev is not None and not (e == "pe" and id(ev[0]) == own):
                self._wait(e, ev)
            if b.x:
                for ev in b.r:
                    if id(ev[0]) != own:
                        self._wait(e, ev)
        for b in writes:
            ev = b.w
            if ev is not None and not (e == "pe" and id(ev[0]) == own):
                self._wait(e, ev)
            for ev in b.r:
                if not (e == "pe" and id(ev[0]) == own):
                    self._wait(e, ev)

    def _mark(self, ev, reads, writes):
        for b in reads:
            b.r.append(ev)
            if len(b.r) > 16:
                d = {}
                for s, v in b.r:
                    if id(s) not in d or d[id(s)][1] < v:
                        d[id(s)] = (s, v)
                b.r = list(d.values())
        for b in writes:
            b.w = ev
            b.r = []

    def op(self, e, fn, reads=(), writes=()):
        self.nops += 1
        if self.limit is not None and self.nops > self.limit:
            return None
        self._deps(e, reads, writes)
        self.cnt[e] += 1
        self.prog[e].append(("i", fn, self.psem[e], 1))
        ev = (self.psem[e], self.cnt[e])
        self._mark(ev, reads, writes)
        return ev

    def dma(self, e, fn, reads=(), writes=()):
        self.nops += 1
        if self.limit is not None and self.nops > self.limit:
            return None
        self._deps(e, reads, writes)
        if e == "pool":
            i = self.swnext
            self.swnext = (self.swnext + 1) % self.nsw
        else:
            i = self.nsw + self.dnext
            self.dnext = (self.dnext + 1) % (len(self.dsems) - self.nsw)
        sem = self.dsems[i]
        if self.dval[i] > 0:
            self._wait(e, (sem, self.dval[i]))
        self.dval[i] += 16
        self.prog[e].append(("i", fn, sem, 16))
        ev = (sem, self.dval[i])
        self._mark(ev, reads, writes)
        return ev

    def barrier(self, engines=None):
        for e in (engines or self.eng):
            for f in self.eng:
                if f != e and self.cnt[f] > 0:
                    self._wait(e, (self.psem[f], self.cnt[f]))
            for i, sm in enumerate(self.dsems):
                if self.dval[i] > 0:
                    self._wait(e, (sm, self.dval[i]))

    def finish(self):
        self.barrier(["sp"])
        nc, prog = self.nc, self.prog

        def emit(engine, lst):
            for it in lst:
                if it[0] == "w":
                    engine.wait_ge(it[1], it[2])
                else:
                    it[1](engine).then_inc(it[2], it[3])

        with nc.Block() as block:
            @block.sync
            def _(x):
                emit(x, prog["sp"])

            @block.tensor
            def _(x):
                emit(x, prog["pe"])

            @block.vector
            def _(x):
                emit(x, prog["dve"])

            @block.scalar
            def _(x):
                emit(x, prog["act"])

            @block.gpsimd
            def _(x):
                emit(x, prog["pool"])


PRM = {}
_o = 0
for _n, _w in [("g1c", 8), ("gq_bc", 64), ("gk_bc", 64), ("gq_col", 1), ("gqp_col", 1), ("gk_col", 1), ("gkp_col", 1),
               ("convw", 16), ("convb", 4), ("ba", 8), ("bi", 8), ("lam", 8), ("gao", 4), ("glo", 4), ("brt", 32),
               ("g2", 1024), ("gf", 1024), ("bg", 256), ("bu", 256)]:
    PRM[_n] = (_o, _w)
    _o += _w
NPRM = _o
CST = {}
_o = 0
for _n, _w in [("identf", 128), ("ustrict", 128), ("onesf", 128), ("rtab", 384), ("msk", 8), ("iota1", 32), ("slot1", 32)]:
    CST[_n] = (_o, _w)
    _o += _w
NCST = _o


def _esz(dt):
    return 2 if dt == BF16 else 4


def build_nc(dbg=False, stop=None, limit=None):
    nc = bass.Bass("TRN2", target_bir_lowering=False)

    def din(name, shape, dt=F32):
        return nc.dram_tensor(name, list(shape), dt, kind="ExternalInput").ap()

    xs = din("xs", [S, D])
    prm_d = din("prm", [128, NPRM])
    cst_d = din("cst", [128, NCST])
    cmat_d = din("cmat", [128, 384])
    w_in_d = din("w_in", [D, 1792])
    wabd_d = din("wabd", [128, 1024])
    wibd_d = din("wibd", [128, 1024])
    w_out_d = din("w_out", [D, D])
    w_router_d = din("w_router", [D, NE])
    if stop is None or stop == "E":
        w_gate_d = din("w_gate", [NE, D, D])
        w_up_d = din("w_up", [NE, D, D])
        w_down_d = din("w_down", [NE, D, D])
        b_down_d = din("b_down", [NE, D])
    y_d = nc.dram_tensor("y", [2048, D], F32, kind="ExternalOutput").ap()
    scr_lru = nc.dram_tensor("scr_lru", [4, 128, S + 4], BF16, kind="Internal").ap()
    scr_gg = nc.dram_tensor("scr_gg", [4, 128, 2048], F32, kind="Internal").ap()
    scr_x1 = nc.dram_tensor("scr_x1", [2048, D], F32, kind="Internal").ap()
    scr_xd = nc.dram_tensor("scr_xd", [NSLOT, D], F32, kind="Internal").ap()
    scr_y = nc.dram_tensor("scr_y", [NSLOT, D], F32, kind="Internal").ap()
    dbg_out = {}
    if dbg:
        dbg_out["d_kT"] = nc.dram_tensor("d_kT", [128, S], BF16, kind="ExternalOutput").ap()
        dbg_out["d_qT"] = nc.dram_tensor("d_qT", [128, 4 * 2048], BF16, kind="ExternalOutput").ap()
        dbg_out["d_V"] = nc.dram_tensor("d_V", [128, 64 * 130], BF16, kind="ExternalOutput").ap()
        dbg_out["d_attn"] = nc.dram_tensor("d_attn", [128, 16 * 512], F32, kind="ExternalOutput").ap()
        dbg_out["d_lru"] = nc.dram_tensor("d_lru", [128, 4 * 2048], BF16, kind="ExternalOutput").ap()
        dbg_out["d_x1"] = nc.dram_tensor("d_x1", [2048, D], F32, kind="ExternalOutput").ap()

    with ExitStack() as ctx:
        fw = FW(nc, ctx)
        fw.limit = limit
        nc._fw = fw
        SB = ctx.enter_context(nc.sbuf_tensor("SB", [128, SB_BYTES // 4], F32))
        pb = [ctx.enter_context(nc.psum_tensor("pb%d" % i, [128, 512], F32))[:] for i in range(8)]
        pbb = [Buf(True) for _ in range(8)]

        def V(off, shape, dt=F32):
            n = int(np.prod(shape[1:]))
            nb = n * _esz(dt)
            assert off % 4 == 0 and nb % 4 == 0 and off + nb <= SB_BYTES, (off, shape)
            ap = SB[:, off // 4:(off + nb) // 4]
            if dt != F32:
                ap = ap.bitcast(dt)
            if len(shape) > 2:
                names = "abcd"[:len(shape) - 1]
                ap = ap.rearrange("p (%s) -> p %s" % (" ".join(names), " ".join(names)),
                                  **{k: int(s) for k, s in zip(names, shape[1:])})
            return ap

        def ACT(out, in_, func, reads, writes, scale=None, bias=None, accum=None):
            kw = {}
            if scale is not None:
                kw["scale"] = scale
            if bias is not None:
                kw["bias"] = bias
            if accum is not None:
                kw["accum_out"] = accum
            return fw.op("act", lambda e: e.activation(out=out, in_=in_, func=func, **kw), reads, writes)

        def TS(out, in0, s1, op0, reads, writes, s2=None, op1=None, eng="dve", accum=None):
            kw = {}
            if op1 is not None:
                kw["op1"] = op1
            if accum is not None:
                kw["accum_out"] = accum
            return fw.op(eng, lambda e: e.tensor_scalar(out=out, in0=in0, scalar1=s1, scalar2=s2, op0=op0, **kw), reads, writes)

        def TT(out, in0, in1, op, reads, writes, eng="dve"):
            return fw.op(eng, lambda e: e.tensor_tensor(out=out, in0=in0, in1=in1, op=op), reads, writes)

        def STT(out, in0, scalar, in1, op0, op1, reads, writes, accum=None):
            kw = {}
            if accum is not None:
                kw["accum_out"] = accum
            return fw.op("dve", lambda e: e.scalar_tensor_tensor(out=out, in0=in0, scalar=scalar, in1=in1, op0=op0, op1=op1, **kw),
                         reads, writes)

        def CP(eng, out, in_, reads, writes):
            if eng == "act":
                return fw.op("act", lambda e: e.copy(out=out, in_=in_), reads, writes)
            return fw.op(eng, lambda e: e.tensor_copy(out=out, in_=in_), reads, writes)

        def MM(out, lhsT, rhs, start, stop, reads, writes, skip=False):
            return fw.op("pe", lambda e: e.matmul(out, lhsT=lhsT, rhs=rhs, start=start, stop=stop, skip_group_check=skip), reads, writes)

        def TR(out, in_, ident, reads, writes):
            return fw.op("pe", lambda e: e.transpose(out=out, in_=in_, identity=ident), reads, writes)

        def DMA(eng, out, in_, reads, writes, slow=False):
            if slow:
                return fw.dma(eng, lambda e: e.dma_start(out=out, in_=in_, allow_slow_non_contiguous=True), reads, writes)
            return fw.dma(eng, lambda e: e.dma_start(out=out, in_=in_), reads, writes)

        def RSTD(out, ssq_ap, inv_n, eps, tmp, reads, writes):
            ACT(tmp, ssq_ap, AF.Ln, reads, writes, scale=inv_n, bias=eps)
            ACT(out, tmp, AF.Exp, writes, writes, scale=-0.5)

        o = 0
        prm = V(o, [128, NPRM]); o += NPRM * 4; prm_b = Buf()
        cst = V(o, [128, NCST]); o += NCST * 4; cst_b = Buf()
        cmat = V(o, [128, 384], BF16); o += 768; cmat_b = Buf()
        small = V(o, [128, 256]); o += 1024; small_b = Buf()
        colt = V(o, [128, 4, 64]); o += 1024; colt_b = Buf()
        diagw = V(o, [128, 16, 128], BF16); o += 4096; diagw_b = Buf()
        wabd = V(o, [128, 8, 128], BF16); o += 2048
        wibd = V(o, [128, 8, 128], BF16); o += 2048; wbd_b = Buf()
        wr_f = V(o, [128, 8, NE]); o += 1024; wr_b = Buf()
        carry = V(o, [128, 8]); o += 32; carry_b = [Buf() for _ in range(8)]
        rstd_a = V(o, [128, 16]); o += 64; rstda_b = Buf()
        cm = V(o, [128, NE]); o += 128; cm_b = Buf()
        gate4 = V(o, [128, 16, 4]); o += 256; gate4_b = Buf()
        dest4 = V(o, [128, 16, 4], I32); o += 256; dest4_b = Buf()
        ones_bf = V(o, [128, 2], BF16); o += 4; ones_b = Buf()
        o = (o + 31) // 32 * 32
        junk = V(o, [128, 1024], BF16); o += 2048; junk_b = Buf()
        sc = V(o, [128, 64]); o += 256
        sc_b = [Buf() for _ in range(64)]
        PH0 = (o + 63) // 64 * 64

        def P(name):
            a, w = PRM[name]
            return prm[:, a:a + w]

        def Cc(name):
            a, w = CST[name]
            return cst[:, a:a + w]

        identf = Cc("identf")
        ident_bf = cmat[:, 0:128]
        perm_bf = cmat[:, 128:256]
        bones_bf = cmat[:, 256:384]
        rtab = Cc("rtab")
        msk = Cc("msk")

        DMA("sp", prm, prm_d[:, :], [], [prm_b])
        DMA("sp", cst, cst_d[:, :], [], [cst_b])
        DMA("pool", cmat, cmat_d[:, :], [], [cmat_b])
        DMA("pool", wabd.rearrange("p a b -> p (a b)"), wabd_d[:, :], [], [wbd_b])
        DMA("pool", wibd.rearrange("p a b -> p (a b)"), wibd_d[:, :], [], [wbd_b])
        DMA("sp", wr_f, w_router_d.rearrange("(c p) e -> p c e", p=128), [], [wr_b])
        fw.op("dve", lambda e: e.memset(ones_bf, 1.0), [], [ones_b])
        fw.op("dve", lambda e: e.memset(cm, 0.0), [], [cm_b])
        fw.op("dve", lambda e: e.memset(carry, 0.0), [], carry_b)

        SM = {}
        so = 0
        for n_, w_ in [("cpar", 8), ("chalf", 8), ("bah", 8), ("bih", 8), ("negM", 1), ("mq", 1), ("mk", 1), ("t8", 8), ("lnh", 1)]:
            SM[n_] = small[:, so:so + w_]
            so += w_
        ACT(SM["t8"], P("lam"), AF.Exp, [prm_b], [small_b], scale=-1.0)
        ACT(SM["t8"], SM["t8"], AF.Ln, [small_b], [small_b], bias=1.0)
        TS(SM["cpar"], SM["t8"], -8.0, ALU.mult, [small_b], [small_b])
        TS(SM["chalf"], SM["t8"], -4.0, ALU.mult, [small_b], [small_b])
        TS(SM["bah"], P("ba"), 0.5, ALU.mult, [prm_b], [small_b])
        TS(SM["bih"], P("bi"), 0.5, ALU.mult, [prm_b], [small_b])
        fw.op("dve", lambda e: e.tensor_reduce(out=SM["mq"], in_=P("gq_bc"), axis=AX.X, op=ALU.max, apply_absolute_value=True), [prm_b], [small_b])
        fw.op("dve", lambda e: e.tensor_reduce(out=SM["mk"], in_=P("gk_bc"), axis=AX.X, op=ALU.max, apply_absolute_value=True), [prm_b], [small_b])
        TT(SM["negM"], SM["mq"], SM["mk"], ALU.mult, [small_b], [small_b])
        TS(SM["negM"], SM["negM"], -8.0, ALU.mult, [small_b], [small_b])
        cosc = rtab[:, 256:320]
        sinc = rtab[:, 320:384]
        TS(colt[:, 0, :], cosc, P("gq_col"), ALU.mult, [prm_b, cst_b], [colt_b])
        TS(colt[:, 1, :], sinc, P("gqp_col"), ALU.mult, [prm_b, cst_b], [colt_b])
        TS(colt[:, 2, :], cosc, P("gk_col"), ALU.mult, [prm_b, cst_b], [colt_b])
        TS(colt[:, 3, :], sinc, P("gkp_col"), ALU.mult, [prm_b, cst_b], [colt_b])
        for ch in range(4):
            for tap in range(4):
                TS(diagw[:, ch * 4 + tap, :], ident_bf, P("convw")[:, ch * 4 + tap:ch * 4 + tap + 1], ALU.mult, [prm_b, cmat_b], [diagw_b])

        _breg = {}

        def breg(e):
            if 'r' not in _breg:
                _breg['r'] = e.to_reg(NSLOT - 1)
            return _breg['r']

        scn = [0]

        def newsc():
            i = scn[0] % 64
            scn[0] += 1
            return sc[:, i:i + 1], sc_b[i]

        o = PH0
        kT = V(o, [128, S], BF16); o += 2 * S; kT_b = Buf()
        Vaug = V(o, [128, 64, 2, 65], BF16); o += 64 * 130 * 2; Vaug_b = Buf()
        qT = V(o, [128, 4, 2048], BF16); o += 16384; qT_b = Buf()
        o = (o + 63) // 64 * 64
        PHA = o
        Win = V(o, [128, 8, 1792], BF16); o += 8 * 1792 * 2; Win_b = Buf()
        xt = []
        for i in range(3):
            xt.append((V(o, [128, D]), Buf())); o += 4096
        xT = []
        for i in range(2):
            xT.append((V(o, [128, 8, 512], BF16), Buf())); o += 8192
        q_bf = V(o, [128, 512], BF16); o += 1024; qbf_b = Buf()
        sq_bf = V(o, [128, 512], BF16); o += 1024; sqbf_b = Buf()
        lnr = V(o, [128, 512]); o += 2048; lnr_b = Buf()
        TAq = V(o, [128, 8, 64]); o += 2048
        TBq = V(o, [128, 8, 64]); o += 2048; Tq_b = Buf()
        TAk = V(o, [128, 8, 64]); o += 2048
        TBk = V(o, [128, 8, 64]); o += 2048; Tk_b = Buf()
        t1 = V(o, [128, 512]); o += 2048; t1_b = Buf()
        t2 = V(o, [128, 512]); o += 2048; t2_b = Buf()
        vT_f = V(o, [128, 512]); o += 2048; vT_b = Buf()
        lst = []
        for i in range(2):
            lst.append((V(o, [128, 4, 512], BF16), Buf())); o += 4096
        ggst = []
        for i in range(2):
            ggst.append((V(o, [128, 4, 512]), Buf())); o += 8192
        wst = [(ggst[0][0].rearrange("p a b -> p (a b)")[:, 0:1792], ggst[0][1]),
               (ggst[1][0].rearrange("p a b -> p (a b)")[:, 0:1792], ggst[1][1])]

        for dc in range(8):
            ws, wsb = wst[dc % 2]
            DMA("sp", ws, w_in_d[dc * 128:(dc + 1) * 128, :], [], [wsb])
            g1 = P("g1c")[:, dc:dc + 1]
            TS(Win[:, dc, 0:512].rearrange("p (j h d) -> p h j d", j=4, h=2),
               ws[:, 0:512].rearrange("p (h j d) -> p h j d", h=2, j=4), g1, ALU.mult, [wsb, prm_b], [Win_b])
            TS(Win[:, dc, 512:1792], ws[:, 512:1792], g1, ALU.mult, [wsb, prm_b], [Win_b])
        fw.op("pool", lambda e: e.memset(Vaug[:, :, :, 64:65], 1.0), [], [Vaug_b])

        cosr = rtab[:, 0:128]
        sinr = rtab[:, 128:256]
        scr_lru_v = scr_lru.rearrange("c p t -> p c t")
        scr_gg_v = scr_gg.rearrange("c p t -> p c t")
        scrl_b = Buf()
        scrg_b = Buf()

        def rope(Pb, Pbb, TA, TB, Tb, outap, outb):
            CP("act", q_bf, Pb, [Pbb], [qbf_b])
            ACT(sq_bf, Pb, AF.Square, [Pbb], [sqbf_b])
            MM(pb[6], perm_bf, q_bf, True, True, [cmat_b, qbf_b], [pbb[6]])
            MM(pb[7], bones_bf, sq_bf, True, True, [cmat_b, sqbf_b], [pbb[7]])
            ACT(t2, pb[7], AF.Ln, [pbb[7]], [t2_b], scale=1.0 / 64, bias=1e-6)
            ACT(lnr, t2, AF.Exp, [t2_b], [lnr_b], scale=-0.5)
            if stop == "X1":
                CP("dve", t1, Pb, [Pbb, Tb], [t1_b])
            elif stop == "X2":
                TT(t1, Pb, lnr, ALU.mult, [Pbb, lnr_b], [t1_b])
            else:
                TT(t1, Pb, TA.rearrange("p a b -> p (a b)"), ALU.mult, [Pbb, Tb], [t1_b])
            TT(t2, pb[6], TB.rearrange("p a b -> p (a b)"), ALU.mult, [pbb[6], Tb], [t2_b])
            TT(t1, t1, t2, ALU.add, [t1_b, t2_b], [t1_b])
            TT(outap, t1, lnr, ALU.mult, [t1_b, lnr_b], [outb])

        def proj(acc_i, c0, xTi):
            xTa, xTbuf = xT[xTi]
            for dc in range(8):
                MM(pb[acc_i], Win[:, dc, c0:c0 + 128], xTa[:, dc, :], dc == 0, dc == 7, [Win_b, xTbuf], [pbb[acc_i]])

        acc_rr = [0]

        def nextacc():
            acc_rr[0] ^= 1
            return 4 + acc_rr[0]

        if stop == "S":
            fw.finish()
            return nc
        for s in range(4):
            for j in range(4):
                blk = s * 4 + j
                if stop == "A0" and blk == 1:
                    fw.finish()
                    return nc
                xTi = blk % 2
                xTa, xTbuf = xT[xTi]
                for t in range(4):
                    tile = blk * 4 + t
                    xa, xb = xt[tile % 3]
                    DMA("sp", xa, xs[tile * 128:(tile + 1) * 128, :], [], [xb])
                    ssq, ssqb = newsc()
                    ACT(junk, xa, AF.Square, [xb], [junk_b, ssqb], accum=ssq)
                    tmp, tmpb = newsc()
                    rs, rsb = newsc()
                    RSTD(rs, ssq, 1.0 / D, 1e-5, tmp, [ssqb], [tmpb, rsb])
                    TS(xa, xa, rs, ALU.mult, [xb, rsb], [xb])
                    for half in range(2):
                        bi = 2 * (tile % 2) + half
                        for q in range(4):
                            dc = half * 4 + q
                            TR(pb[bi][:, q * 128:(q + 1) * 128], xa[:, dc * 128:(dc + 1) * 128], identf, [xb, cst_b], [pbb[bi]])
                        CP("act" if half == 0 else "dve", xTa[:, half * 4:(half + 1) * 4, t * 128:(t + 1) * 128],
                           pb[bi].rearrange("p (a b) -> p a b", a=4), [pbb[bi]], [xTbuf])
                if stop == "A0a":
                    fw.finish()
                    return nc
                r0 = blk * 8
                TT(TAk, cosr[:, r0:r0 + 8].unsqueeze(2).to_broadcast([128, 8, 64]), colt[:, 2, :].unsqueeze(1).to_broadcast([128, 8, 64]),
                   ALU.mult, [cst_b, colt_b], [Tk_b])
                TT(TBk, sinr[:, r0:r0 + 8].unsqueeze(2).to_broadcast([128, 8, 64]), colt[:, 3, :].unsqueeze(1).to_broadcast([128, 8, 64]),
                   ALU.mult, [cst_b, colt_b], [Tk_b])
                a = nextacc()
                proj(a, 512, xTi)
                rope(pb[a], pbb[a], TAk, TBk, Tk_b, kT[:, blk * 512:(blk + 1) * 512], kT_b)
                if stop in ("A0b", "X1", "X2"):
                    fw.finish()
                    return nc
                a = nextacc()
                proj(a, 640, xTi)
                CP("act", vT_f, pb[a], [pbb[a]], [vT_b])
                for t in range(4):
                    TR(pb[6][:, t * 128:(t + 1) * 128], vT_f[:, t * 128:(t + 1) * 128], identf, [vT_b, cst_b], [pbb[6]])
                CP("dve", Vaug[:, blk * 4:(blk + 1) * 4, :, 0:64], pb[6].rearrange("p (t h d) -> p t h d", t=4, h=2), [pbb[6]], [Vaug_b])
                if stop == "A0c":
                    fw.finish()
                    return nc
                la, lb = lst[blk % 2]
                for cc in range(4):
                    a = nextacc()
                    proj(a, 768 + cc * 128, xTi)
                    CP("act" if cc % 2 == 0 else "dve", la[:, cc, :], pb[a], [pbb[a]], [lb])
                DMA("sp", scr_lru_v[:, :, 2 + blk * 512:2 + (blk + 1) * 512], la, [lb], [scrl_b])
                if blk == 15:
                    DMA("sp", scr_lru_v[:, :, 0:2], la[:, :, 510:512], [lb], [scrl_b], slow=True)
                if blk == 0:
                    DMA("sp", scr_lru_v[:, :, S + 2:S + 3], la[:, :, 0:1], [lb], [scrl_b], slow=True)
                if stop == "A0d":
                    fw.finish()
                    return nc
                if s == 0:
                    TT(TAq, cosr[:, r0:r0 + 8].unsqueeze(2).to_broadcast([128, 8, 64]), colt[:, 0, :].unsqueeze(1).to_broadcast([128, 8, 64]),
                       ALU.mult, [cst_b, colt_b], [Tq_b])
                    TT(TBq, sinr[:, r0:r0 + 8].unsqueeze(2).to_broadcast([128, 8, 64]), colt[:, 1, :].unsqueeze(1).to_broadcast([128, 8, 64]),
                       ALU.mult, [cst_b, colt_b], [Tq_b])
                    for pr in range(4):
                        a = nextacc()
                        proj(a, pr * 128, xTi)
                        rope(pb[a], pbb[a], TAq, TBq, Tq_b, qT[:, pr, j * 512:(j + 1) * 512], qT_b)
                    ga, gb = ggst[blk % 2]
                    for cc in range(4):
                        a = nextacc()
                        proj(a, 1280 + cc * 128, xTi)
                        ACT(t1, pb[a], AF.Square, [pbb[a]], [t1_b])
                        CP("act", t2, pb[a], [pbb[a]], [t2_b])
                        TS(t1, t1, 0.044715, ALU.mult, [t1_b], [t1_b], s2=1.0, op1=ALU.add)
                        TT(t1, t1, t2, ALU.mult, [t1_b, t2_b], [t1_b])
                        ACT(lnr, t1, AF.Sigmoid, [t1_b], [lnr_b], scale=2.0 * math.sqrt(2.0 / math.pi))
                        TT(ga[:, cc, :], lnr, t2, ALU.mult, [lnr_b, t2_b], [gb])
                    DMA("sp", scr_gg_v[:, :, j * 512:(j + 1) * 512], ga, [gb], [scrg_b])

        if dbg:
            DMA("sp", dbg_out["d_kT"][:, :], kT, [kT_b], [Buf()])
            DMA("sp", dbg_out["d_qT"][:, :], qT.rearrange("p a b -> p (a b)"), [qT_b], [Buf()])
            DMA("sp", dbg_out["d_V"][:, :], Vaug.rearrange("p a b c -> p (a b c)"), [Vaug_b], [Buf()])
        if stop == "A":
            fw.finish()
            return nc
        fw.barrier()

        o = PHA
        attn_raw = V(o, [128, 16, 512]); o += 32768; attn_b = Buf()
        pT = []
        for i in range(3):
            pT.append((V(o, [128, 512], BF16), Buf())); o += 1024
        rc = V(o, [128, 4]); o += 16; rc_b = Buf()
        o = (o + 63) // 64 * 64
        aT_off = o
        aT_bf = V(o, [128, 16, 4, 128], BF16); o += 16384; aT_b = Buf()
        negM = SM["negM"]
        it = 0
        for qb in range(4):
            for h in range(8):
                pr, kv = h % 4, h // 4
                rows = slice(kv * 64, (kv + 1) * 64)
                oi = 3 + (it % 2)
                it += 1
                for kc in range(64):
                    si = kc % 3
                    pa, pab = pT[si]
                    MM(pb[si], kT[rows, kc * 128:(kc + 1) * 128], qT[rows, pr, qb * 512:(qb + 1) * 512], True, True, [kT_b, qT_b], [pbb[si]])
                    ACT(pa, pb[si], AF.Exp, [pbb[si], small_b], [pab], scale=0.125, bias=negM)
                    for qt in range(4):
                        MM(pb[oi][:, qt * 65:(qt + 1) * 65], pa[:, qt * 128:(qt + 1) * 128], Vaug[:, kc, kv, :],
                           kc == 0 and qt == 0, kc == 63, [pab, Vaug_b], [pbb[oi]], skip=True)
                ov = pb[oi][:, 0:260].rearrange("p (a b) -> p a b", a=4)
                fw.op("dve", lambda e, ov=ov: e.reciprocal(out=rc, in_=ov[:, :, 64]), [pbb[oi]], [rc_b])
                for qt in range(4):
                    TS(attn_raw[:, qb * 4 + qt, h * 64:(h + 1) * 64], ov[:, qt, 0:64], rc[:, qt:qt + 1], ALU.mult, [pbb[oi], rc_b], [attn_b])
            for qt in range(4):
                tile = qb * 4 + qt
                ssq, ssqb = newsc()
                ACT(junk[:, 0:512], attn_raw[:, tile, :], AF.Square, [attn_b], [junk_b, ssqb], accum=ssq)
                tmp, tmpb = newsc()
                RSTD(rstd_a[:, tile:tile + 1], ssq, 1.0 / 512, 1e-5, tmp, [ssqb], [tmpb, rstda_b])
                for cc in range(4):
                    TR(pb[5][:, cc * 128:(cc + 1) * 128], attn_raw[:, tile, cc * 128:(cc + 1) * 128], identf, [attn_b, cst_b], [pbb[5]])
                CP("dve", aT_bf[:, tile, :, :], pb[5].rearrange("p (a b) -> p a b", a=4), [pbb[5]], [aT_b])
        if dbg:
            DMA("sp", dbg_out["d_attn"][:, :], attn_raw.rearrange("p a b -> p (a b)"), [attn_b], [Buf()])
        if stop == "B":
            fw.finish()
            return nc
        fw.barrier()

        o = PH0
        lru_acc = V(o, [128, 4, 2048]); o += 32768; lacc_b = [Buf() for _ in range(4)]
        lruT_off = o
        lruT_bf = V(o, [128, 4, 2048], BF16); o += 16384; lruT_b = Buf()
        regs = [(o, aT_off), (aT_off + 16384, SB_BYTES)]
        ptr = [regs[0][0], 0]

        def lalloc(nbytes):
            if ptr[0] + nbytes > regs[ptr[1]][1]:
                ptr[1] += 1
                ptr[0] = regs[ptr[1]][0]
            r = ptr[0]
            ptr[0] += (nbytes + 63) // 64 * 64
            assert ptr[0] <= regs[ptr[1]][1]
            return r

        xl = [(V(lalloc(4112), [128, 2056], BF16), Buf()) for _ in range(2)]
        xc_f = V(lalloc(8192), [128, 2048]); xcf_b = Buf()
        xc_b = V(lalloc(4096), [128, 2048], BF16); xcb_b = Buf()
        th_a = V(lalloc(8192), [128, 2048]); tha_b = Buf()
        th_i = V(lalloc(8192), [128, 2048]); thi_b = Buf()
        a_t = V(lalloc(8192), [128, 2048]); at_b = Buf()
        tmp_t = V(lalloc(8192), [128, 2048]); tmpt_b = Buf()
        bb_t = V(lalloc(8192), [128, 2048]); bbt_b = Buf()
        hh_t = V(lalloc(8192), [128, 2048]); hht_b = Buf()
        gg_t = xc_f; ggt_b = xcf_b
        LN_HALF = math.log(0.5)
        n_unit = 0
        for d_ in range(2):
            order = [1, 2, 3, 0] if d_ == 0 else [3, 2, 1, 0]
            for slot in order:
                for ch in range(4):
                    xla, xlb = xl[n_unit % 2]
                    n_unit += 1
                    DMA("sp", xla[:, 0:2051], scr_lru[ch, :, slot * 2048:slot * 2048 + 2051], [scrl_b], [xlb])
                    TS(xla[:, 0:2], xla[:, 0:2], msk[:, slot:slot + 1], ALU.mult, [xlb, cst_b], [xlb])
                    TS(xla[:, 2050:2051], xla[:, 2050:2051], msk[:, 4 + slot:5 + slot], ALU.mult, [xlb, cst_b], [xlb])
                    pidx = d_ * 4 + ch
                    for jb in range(4):
                        cs = slice(jb * 512, (jb + 1) * 512)
                        for tap in range(4):
                            MM(pb[0], diagw[:, ch * 4 + tap, :], xla[:, jb * 512 + tap:jb * 512 + tap + 512], tap == 0, tap == 3,
                               [diagw_b, xlb], [pbb[0]])
                        ACT(xc_f[:, cs], pb[0], AF.Identity, [pbb[0], prm_b], [xcf_b], bias=P("convb")[:, ch:ch + 1])
                        TS(xc_b[:, cs], pb[0], P("convb")[:, ch:ch + 1], ALU.add, [pbb[0], prm_b], [xcb_b])
                        MM(pb[1], wabd[:, pidx, :], xc_b[:, cs], True, True, [wbd_b, xcb_b], [pbb[1]])
                        MM(pb[2], wibd[:, pidx, :], xc_b[:, cs], True, True, [wbd_b, xcb_b], [pbb[2]])
                        ACT(th_a[:, cs], pb[1], AF.Tanh, [pbb[1], small_b], [tha_b], scale=0.5, bias=SM["bah"][:, pidx:pidx + 1])
                        ACT(th_i[:, cs], pb[2], AF.Tanh, [pbb[2], small_b], [thi_b], scale=0.5, bias=SM["bih"][:, pidx:pidx + 1])
                    ch_ = SM["chalf"][:, pidx:pidx + 1]
                    c_ = SM["cpar"][:, pidx:pidx + 1]
                    ACT(a_t, th_a, AF.Exp, [tha_b, small_b], [at_b], scale=ch_, bias=ch_)
                    ACT(tmp_t, th_a, AF.Exp, [tha_b, small_b], [tmpt_b], scale=c_, bias=c_)
                    ACT(bb_t, tmp_t, AF.Ln, [tmpt_b], [bbt_b], scale=-1.0, bias=1.0)
                    ACT(tmp_t, bb_t, AF.Exp, [bbt_b], [tmpt_b], scale=0.5, bias=LN_HALF)
                    TT(tmp_t, tmp_t, xc_f, ALU.mult, [tmpt_b, xcf_b], [tmpt_b])
                    STT(bb_t, th_i, 1.0, tmp_t, ALU.add, ALU.mult, [thi_b, tmpt_b], [bbt_b])
                    cidx = d_ * 4 + ch
                    cb = carry_b[cidx]
                    ini, inib = newsc()
                    mcol = msk[:, slot:slot + 1] if d_ == 0 else msk[:, 4 + slot:5 + slot]
                    TS(ini, carry[:, cidx:cidx + 1], mcol, ALU.mult, [cb, cst_b], [inib])
                    last = slot == 0
                    if last and d_ == 0:
                        dst, dstb = lru_acc[:, ch, :], lacc_b[ch]
                    else:
                        dst, dstb = hh_t, hht_b
                    if d_ == 0:
                        fw.op("dve", lambda e, dst=dst, ini=ini: e.tensor_tensor_scan(out=dst, data0=a_t, data1=bb_t, initial=ini,
                                                                                      op0=ALU.mult, op1=ALU.add),
                              [at_b, bbt_b, inib], [dstb])
                        CP("dve", carry[:, cidx:cidx + 1], dst[:, 2047:2048], [dstb], [cb])
                    else:
                        fw.op("dve", lambda e, dst=dst, ini=ini: e.tensor_tensor_scan(out=dst[:, ::-1], data0=a_t[:, ::-1], data1=bb_t[:, ::-1],
                                                                                      initial=ini, op0=ALU.mult, op1=ALU.add),
                              [at_b, bbt_b, inib], [dstb])
                        CP("dve", carry[:, cidx:cidx + 1], dst[:, 0:1], [dstb], [cb])
                    if last and d_ == 1:
                        TT(lru_acc[:, ch, :], lru_acc[:, ch, :], hh_t, ALU.add, [lacc_b[ch], hht_b], [lacc_b[ch]], eng="pool")
                        DMA("sp", gg_t, scr_gg[ch, :, :], [scrg_b], [ggt_b])
                        TT(lruT_bf[:, ch, :], lru_acc[:, ch, :], gg_t, ALU.mult, [lacc_b[ch], ggt_b], [lruT_b], eng="pool")
        if dbg:
            DMA("sp", dbg_out["d_lru"][:, :], lruT_bf.rearrange("p a b -> p (a b)"), [lruT_b], [Buf()])
        if stop == "C":
            fw.finish()
            return nc
        fw.barrier()

        o = PH0
        wout = V(o, [128, 8, D], BF16); o += 16384; wout_b = Buf()
        assert o <= lruT_off
        regs = [(lruT_off + 16384, aT_off), (aT_off + 16384, SB_BYTES)]
        ptr = [regs[0][0], 0]
        xq = [(V(lalloc(4096), [128, D]), Buf()) for _ in range(2)]
        xn2 = [(V(lalloc(4096), [128, D]), Buf()) for _ in range(2)]
        PB2 = []
        for _par in range(2):
            dct = {}
            dct["xn2T"] = (V(lalloc(4096), [128, 8, 128]), Buf())
            dct["sqt"] = (V(lalloc(1024), [128, 4, 128], BF16), Buf())
            for nm_ in ["lg", "mask", "ex", "g32", "sv", "oh"]:
                dct[nm_] = (V(lalloc(128), [128, NE]), Buf())
            for nm_ in ["v8", "s8", "e8"]:
                dct[nm_] = (V(lalloc(64), [128, 8]), Buf())
            PB2.append(dct)
        wos = [(V(lalloc(4096), [128, D]), Buf()) for _ in range(2)]
        for dc in range(8):
            ws, wsb = wos[dc % 2]
            DMA("sp", ws, w_out_d[dc * 128:(dc + 1) * 128, :], [], [wsb])
            gcol = P("gao")[:, dc:dc + 1] if dc < 4 else P("glo")[:, dc - 4:dc - 3]
            TS(wout[:, dc, :], ws, gcol, ALU.mult, [wsb, prm_b], [wout_b])
        scrx1_b = Buf()
        scrxd_b = Buf()
        ustrict = Cc("ustrict")
        onesf = Cc("onesf")
        iota1 = Cc("iota1")
        slot1 = Cc("slot1")
        for i in range(16):
            xa, xb = xq[i % 2]
            na, nb = xn2[i % 2]
            dct = PB2[i % 2]
            xn2T, xn2T_b = dct["xn2T"]
            sqt, sqt_b = dct["sqt"]
            lg, lg_b = dct["lg"]
            mask, mask_b = dct["mask"]
            ex, ex_b = dct["ex"]
            g32, g32_b = dct["g32"]
            sv, sv_b = dct["sv"]
            oh, oh_b = dct["oh"]
            v8, v8_b = dct["v8"]
            s8, s8_b = dct["s8"]
            e8, e8_b = dct["e8"]
            DMA("sp", xa, xs[i * 128:(i + 1) * 128, :], [], [xb])
            for half in range(2):
                hs = slice(half * 512, (half + 1) * 512)
                for cc in range(4):
                    MM(pb[half], aT_bf[:, i, cc, :], wout[:, cc, hs], cc == 0, cc == 3, [aT_b, wout_b], [pbb[half]])
                for cc in range(4):
                    MM(pb[2 + half], lruT_bf[:, cc, i * 128:(i + 1) * 128], wout[:, 4 + cc, hs], cc == 0, cc == 3, [lruT_b, wout_b], [pbb[2 + half]])
            ACT(sqt, lruT_bf[:, :, i * 128:(i + 1) * 128], AF.Square, [lruT_b], [sqt_b])
            for cc in range(4):
                MM(pb[4][:, 0:2], sqt[:, cc, :], ones_bf, cc == 0, cc == 3, [sqt_b, ones_b], [pbb[4]])
            tmp, tmpb = newsc()
            rl, rlb = newsc()
            RSTD(rl, pb[4][:, 0:1], 1.0 / 512, 1e-5, tmp, [pbb[4]], [tmpb, rlb])
            for half in range(2):
                hs = slice(half * 512, (half + 1) * 512)
                STT(xa[:, hs], pb[half], rstd_a[:, i:i + 1], xa[:, hs], ALU.mult, ALU.add, [pbb[half], rstda_b, xb], [xb])
                STT(xa[:, hs], pb[2 + half], rl, xa[:, hs], ALU.mult, ALU.add, [pbb[2 + half], rlb, xb], [xb])
            DMA("sp", scr_x1[i * 128:(i + 1) * 128, :], xa, [xb], [scrx1_b])
            if dbg:
                DMA("sp", dbg_out["d_x1"][i * 128:(i + 1) * 128, :], xa, [xb], [Buf()])
            ssq, ssqb = newsc()
            ACT(junk, xa, AF.Square, [xb], [junk_b, ssqb], accum=ssq)
            tmp, tmpb = newsc()
            r2, r2b = newsc()
            RSTD(r2, ssq, 1.0 / D, 1e-5, tmp, [ssqb], [tmpb, r2b])
            STT(na, xa, r2, P("g2"), ALU.mult, ALU.mult, [xb, r2b, prm_b], [nb])
            for half in range(2):
                for q in range(4):
                    dc = half * 4 + q
                    TR(pb[5 + half][:, q * 128:(q + 1) * 128], na[:, dc * 128:(dc + 1) * 128], identf, [nb, cst_b], [pbb[5 + half]])
                CP("act", xn2T[:, half * 4:(half + 1) * 4, :], pb[5 + half].rearrange("p (a b) -> p a b", a=4), [pbb[5 + half]], [xn2T_b])
            for dc in range(8):
                MM(pb[7][:, 0:NE], xn2T[:, dc, :], wr_f[:, dc, :], dc == 0, dc == 7, [xn2T_b, wr_b], [pbb[7]])
            TT(lg, pb[7][:, 0:NE], P("brt"), ALU.add, [pbb[7], prm_b], [lg_b])
            fw.op("dve", lambda e, v8=v8, lg=lg: e.max(out=v8, in_=lg), [lg_b], [v8_b])
            TS(mask, lg, v8[:, 3:4], ALU.is_ge, [lg_b, v8_b], [mask_b])
            nv, nvb = newsc()
            TS(nv, v8[:, 0:1], -1.0, ALU.mult, [v8_b], [nvb])
            ACT(ex, lg, AF.Exp, [lg_b, nvb], [ex_b], bias=nv)
            sm_, smb = newsc()
            STT(ex, ex, 1.0, mask, ALU.mult, ALU.mult, [ex_b, mask_b], [ex_b, smb], accum=sm_)
            rs_, rsb = newsc()
            fw.op("dve", lambda e, rs_=rs_, sm_=sm_: e.reciprocal(out=rs_, in_=sm_), [smb], [rsb])
            TS(g32, ex, rs_, ALU.mult, [ex_b, rsb], [g32_b])
            MM(pb[4][:, 32:64], ustrict, mask, True, False, [cst_b, mask_b], [pbb[4]])
            MM(pb[4][:, 32:64], onesf, cm, False, True, [cst_b, cm_b], [pbb[4]])
            TT(sv, pb[4][:, 32:64], slot1, ALU.add, [pbb[4], cst_b], [sv_b])
            TT(sv, sv, mask, ALU.mult, [sv_b, mask_b], [sv_b])
            TT(cm, cm, mask, ALU.add, [cm_b, mask_b], [cm_b])
            fw.op("dve", lambda e, s8=s8, sv=sv: e.max(out=s8, in_=sv), [sv_b], [s8_b])
            TS(dest4[:, i, :], s8[:, 0:4], -1.0, ALU.add, [s8_b], [dest4_b])
            TT(sv, mask, iota1, ALU.mult, [mask_b, cst_b, sv_b], [sv_b])
            fw.op("dve", lambda e, e8=e8, sv=sv: e.max(out=e8, in_=sv), [sv_b], [e8_b])
            for k in range(4):
                TS(oh, iota1, e8[:, k:k + 1], ALU.is_equal, [cst_b, e8_b], [oh_b])
                STT(oh, oh, 1.0, g32, ALU.mult, ALU.mult, [oh_b, g32_b], [oh_b, gate4_b], accum=gate4[:, i, k:k + 1])
            for k in range(4):
                fw.dma("pool", lambda e, na=na, i=i, k=k: e.indirect_dma_start(
                    out=scr_xd[:, :], out_offset=bass.IndirectOffsetOnAxis(ap=dest4[:, i, k:k + 1], axis=0),
                    in_=na, in_offset=None, bounds_check=breg(e), oob_is_err=False), [nb, dest4_b], [scrxd_b])
        if stop == "D":
            fw.finish()
            return nc
        fw.barrier()

        o = PH0
        wring = []
        for i in range(6):
            wring.append((V(o, [128, 8, D], BF16), Buf())); o += 16384
        xe_f = V(o, [128, 4, D]); o += 16384; xef_b = Buf()
        xeT = V(o, [128, 8, SUBC], BF16); o += 8 * SUBC * 2; xeT_b = Buf()
        hT = V(o, [128, 8, SUBC], BF16); o += 8 * SUBC * 2; hT_b = Buf()
        bdb = []
        for i in range(1):
            bdb.append((V(o, [128, D]), Buf())); o += 4096
        yst = []
        for i in range(2):
            yst.append((V(o, [128, D]), Buf())); o += 4096
        gc = V(o, [128, SUBC]); o += SUBC * 4; gc_b = Buf()
        sg = V(o, [128, SUBC]); o += SUBC * 4; sg_b = Buf()
        uc = V(o, [128, SUBC]); o += SUBC * 4; uc_b = Buf()
        assert o <= SB_BYTES, o
        scry_b = Buf()
        bg = P("bg")
        bu = P("bu")
        ny = 0
        for ex_i in range(NE):
            wg, wgb = wring[(ex_i % 2) * 3 + 0]
            wu, wub = wring[(ex_i % 2) * 3 + 1]
            wd, wdb = wring[(ex_i % 2) * 3 + 2]
            DMA("pool", wg, w_gate_d[ex_i].rearrange("(c p) f -> p c f", p=128), [], [wgb])
            DMA("pool", wu, w_up_d[ex_i].rearrange("(c p) f -> p c f", p=128), [], [wub])
            DMA("pool", wd, w_down_d[ex_i].rearrange("(c p) f -> p c f", p=128), [], [wdb])
            bda, bdbb = bdb[0]
            DMA("sp", bda, b_down_d[ex_i:ex_i + 1, :].partition_broadcast(128), [], [bdbb])
            for sbk in range(CAP // SUBC):
                base = ex_i * CAP + sbk * SUBC
                DMA("sp", xe_f, scr_xd[base:base + SUBC, :].rearrange("(t p) d -> p t d", p=128), [scrxd_b], [xef_b])
                for t in range(4):
                    for half in range(2):
                        bi = half
                        for q in range(4):
                            dc = half * 4 + q
                            TR(pb[bi][:, q * 128:(q + 1) * 128], xe_f[:, t, dc * 128:(dc + 1) * 128], identf, [xef_b, cst_b], [pbb[bi]])
                        CP("act", xeT[:, half * 4:(half + 1) * 4, t * 128:(t + 1) * 128], pb[bi].rearrange("p (a b) -> p a b", a=4), [pbb[bi]], [xeT_b])
                for fc in range(8):
                    fs = slice(fc * 128, (fc + 1) * 128)
                    pg, pu = 2 + 2 * (fc % 2), 3 + 2 * (fc % 2)
                    for dc in range(8):
                        MM(pb[pg], wg[:, dc, fs], xeT[:, dc, :], dc == 0, dc == 7, [wgb, xeT_b], [pbb[pg]])
                    for dc in range(8):
                        MM(pb[pu], wu[:, dc, fs], xeT[:, dc, :], dc == 0, dc == 7, [wub, xeT_b], [pbb[pu]])
                    bgc = bg[:, ex_i * 8 + fc:ex_i * 8 + fc + 1]
                    buc = bu[:, ex_i * 8 + fc:ex_i * 8 + fc + 1]
                    TS(gc, pb[pg], bgc, ALU.add, [pbb[pg], prm_b], [gc_b], s2=7.0, op1=ALU.min)
                    ACT(sg, gc, AF.Sigmoid, [gc_b], [sg_b], scale=1.702)
                    TS(uc, pb[pu], buc, ALU.add, [pbb[pu], prm_b], [uc_b], s2=7.0, op1=ALU.min)
                    TS(uc, uc, -7.0, ALU.max, [uc_b], [uc_b], s2=1.0, op1=ALU.add)
                    TT(gc, gc, sg, ALU.mult, [gc_b, sg_b], [gc_b])
                    TT(hT[:, fc, :], uc, gc, ALU.mult, [uc_b, gc_b], [hT_b])
                for t in range(4):
                    ya, yb = yst[ny % 2]
                    ny += 1
                    for half in range(2):
                        hs = slice(half * 512, (half + 1) * 512)
                        pi = 6 + half
                        for fc in range(8):
                            MM(pb[pi], hT[:, fc, t * 128:(t + 1) * 128], wd[:, fc, hs], fc == 0, fc == 7, [hT_b, wdb], [pbb[pi]])
                        TT(ya[:, hs], pb[pi], bda[:, hs], ALU.add, [pbb[pi], bdbb], [yb])
                    DMA("act", scr_y[base + t * 128:base + (t + 1) * 128, :], ya, [yb], [scry_b])
        if stop == "E":
            fw.finish()
            return nc
        fw.barrier()

        o = PH0
        acc = [(V(o + i * 4096, [128, D]), Buf()) for i in range(2)]
        o += 8192
        yk = [(V(o + i * 4096, [128, D]), Buf()) for i in range(4)]
        o += 16384
        ot = [(V(o + i * 4096, [128, D]), Buf()) for i in range(2)]
        yout_b = Buf()
        for i in range(16):
            aa, ab = acc[i % 2]
            DMA("sp", aa, scr_x1[i * 128:(i + 1) * 128, :], [scrx1_b], [ab])
            for k in range(4):
                ya, yb = yk[k]
                fw.dma("pool", lambda e, ya=ya, i=i, k=k: e.indirect_dma_start(
                    out=ya, out_offset=None, in_=scr_y[:, :],
                    in_offset=bass.IndirectOffsetOnAxis(ap=dest4[:, i, k:k + 1], axis=0), bounds_check=breg(e), oob_is_err=False),
                    [scry_b, dest4_b], [yb])
                STT(aa, ya, gate4[:, i, k:k + 1], aa, ALU.mult, ALU.add, [yb, gate4_b, ab], [ab])
            ssq, ssqb = newsc()
            ACT(junk, aa, AF.Square, [ab], [junk_b, ssqb], accum=ssq)
            tmp, tmpb = newsc()
            rf, rfb = newsc()
            RSTD(rf, ssq, 1.0 / D, 1e-5, tmp, [ssqb], [tmpb, rfb])
            oa, ob = ot[i % 2]
            STT(oa, aa, rf, P("gf"), ALU.mult, ALU.mult, [ab, rfb, prm_b], [ob])
            DMA("sp", y_d[i * 128:(i + 1) * 128, :], oa, [ob], [yout_b])
        fw.finish()
    return nc


def _rope_tables(c):
    invf = (10000.0 ** (-np.arange(16, dtype=np.float64) / 16.0))
    tab = np.ones((128, 384), np.float64)
    R = np.arange(128)
    rows = ((c + R // 32) % 4) * 32 + R % 32
    cols = np.arange(64)
    for p in range(128):
        d = p % 64
        sgn = -1.0 if (d % 32) < 16 else 1.0
        f = d % 16
        if d < 32:
            tab[p, 0:128] = np.cos(rows * invf[f])
            tab[p, 128:256] = sgn * np.sin(rows * invf[f])
            tab[p, 256:320] = 1.0
            tab[p, 320:384] = 1.0
        else:
            tab[p, 0:128] = 1.0
            tab[p, 128:256] = 1.0
            tab[p, 256:320] = np.cos(cols * invf[f])
            tab[p, 320:384] = sgn * np.sin(cols * invf[f])
    return tab.astype(np.float32)


def _consts(c):
    cst = np.zeros((128, NCST), np.float32)
    a, w = CST["identf"]; cst[:, a:a + w] = np.eye(128, dtype=np.float32)
    a, w = CST["ustrict"]; cst[:, a:a + w] = np.triu(np.ones((128, 128), np.float32), 1)
    a, w = CST["onesf"]; cst[:, a:a + w] = 1.0
    a, w = CST["rtab"]; cst[:, a:a + w] = _rope_tables(c)
    a, w = CST["msk"]
    for s in range(4):
        cst[:, a + s] = 0.0 if (c + s) % 4 == 0 else 1.0
        cst[:, a + 4 + s] = 0.0 if (c + s) % 4 == 3 else 1.0
    a, w = CST["iota1"]; cst[:, a:a + w] = np.arange(1, 33, dtype=np.float32)[None, :]
    a, w = CST["slot1"]; cst[:, a:a + w] = (np.arange(32, dtype=np.float32) * CAP + 1.0)[None, :]
    cmat = np.zeros((128, 384), np.float32)
    cmat[:, 0:128] = np.eye(128)
    for m in range(128):
        d = m % 64
        base = m - d
        blk = d - d % 32
        i = d % 32
        pi = i + 16 if i < 16 else i - 16
        cmat[base + blk + pi, 128 + m] = 1.0
    cmat[0:64, 256:320] = 1.0
    cmat[64:128, 320:384] = 1.0
    return cst, cmat


def _partner64():
    d = np.arange(64)
    i = d % 32
    return d - i + np.where(i < 16, i + 16, i - 16)


def _prm(inp):
    prm = np.zeros((128, NPRM), np.float32)

    def put(name, arr):
        a, w = PRM[name]
        prm[:, a:a + w] = np.asarray(arr, np.float32).reshape(128, w)

    col = lambda v, n: np.ascontiguousarray(np.asarray(v).reshape(n, 128).T)
    gq = np.asarray(inp["q_norm_g"]).reshape(64)
    gk = np.asarray(inp["k_norm_g"]).reshape(64)
    pp = _partner64()
    put("g1c", col(inp["norm1_g"].reshape(-1), 8))
    put("gq_bc", np.broadcast_to(gq, (128, 64)))
    put("gk_bc", np.broadcast_to(gk, (128, 64)))
    d64 = np.arange(128) % 64
    put("gq_col", gq[d64]); put("gqp_col", gq[pp[d64]]); put("gk_col", gk[d64]); put("gkp_col", gk[pp[d64]])
    cw = np.asarray(inp["conv_w"]).reshape(4, 4, 128)
    put("convw", np.transpose(cw, (2, 1, 0)).reshape(128, 16))
    put("convb", col(inp["conv_b"].reshape(-1), 4))
    for nm, key in [("ba", "lru_ba"), ("bi", "lru_bi"), ("lam", "lru_lam")]:
        v = np.asarray(inp[key]).reshape(2, 4, 128)
        put(nm, np.transpose(v, (2, 0, 1)).reshape(128, 8))
    put("gao", col(inp["attn_out_g"].reshape(-1), 4))
    put("glo", col(inp["lru_out_g"].reshape(-1), 4))
    put("brt", np.broadcast_to(np.asarray(inp["b_router"]).reshape(32), (128, 32)))
    put("g2", np.broadcast_to(np.asarray(inp["norm2_g"]).reshape(1024), (128, 1024)))
    put("gf", np.broadcast_to(np.asarray(inp["final_g"]).reshape(1024), (128, 1024)))
    for nm, key in [("bg", "b_gate"), ("bu", "b_up")]:
        v = np.asarray(inp[key]).reshape(32, 8, 128)
        put(nm, np.transpose(v, (2, 0, 1)).reshape(128, 256))
    return prm


def _blockdiag(w):
    w = np.asarray(w).reshape(2, 4, 2, 64, 64)
    out = np.zeros((128, 2, 4, 128), np.float32)
    for d in range(2):
        for ch in range(4):
            for b in range(2):
                out[b * 64:(b + 1) * 64, d, ch, b * 64:(b + 1) * 64] = w[d, ch, b]
    return out.reshape(128, 1024)


def make_in_maps(inp):
    x = np.asarray(inp["x"], np.float32)
    shared = {
        "prm": _prm(inp),
        "w_in": np.ascontiguousarray(np.asarray(inp["w_in"], np.float32).reshape(D, 1792)),
        "wabd": _blockdiag(inp["lru_wa"]),
        "wibd": _blockdiag(inp["lru_wi"]),
        "w_out": np.ascontiguousarray(np.asarray(inp["w_out"], np.float32).reshape(D, D)),
        "w_router": np.ascontiguousarray(np.asarray(inp["w_router"], np.float32).reshape(D, NE)),
        "w_gate": np.ascontiguousarray(np.asarray(inp["w_gate"], np.float32).reshape(NE, D, D)),
        "w_up": np.ascontiguousarray(np.asarray(inp["w_up"], np.float32).reshape(NE, D, D)),
        "w_down": np.ascontiguousarray(np.asarray(inp["w_down"], np.float32).reshape(NE, D, D)),
        "b_down": np.ascontiguousarray(np.asarray(inp["b_down"], np.float32).reshape(NE, D)),
    }
    maps = []
    for r in range(8):
        b, c = r // 4, r % 4
        order = [(c + s) % 4 for s in range(4)]
        xsr = np.concatenate([x[b, k * 2048:(k + 1) * 2048] for k in order], axis=0)
        cst, cmat = _consts(c)
        m = dict(shared)
        m["xs"] = np.ascontiguousarray(xsr)
        m["cst"] = cst
        m["cmat"] = cmat
        maps.append(m)
    return maps


def kernel(**inputs):
    nc = build_nc()
    in_maps = make_in_maps(inputs)
    res = run_bass_kernel_spmd(nc, in_maps, core_ids=list(range(8)))
    out = np.zeros((2, S, D), np.float32)
    for r in range(8):
        b, c = r // 4, r % 4
        out[b, c * 2048:(c + 1) * 2048] = np.asarray(res.results[r]["y"], np.float32)
    return out
```
